# Optimizing a Trainium2 kernel written in Bass

```python
import math
import jax, jax.numpy as jnp
from jax import lax
import numpy as np

D_MODEL = 2048
BATCH = 16
SEQ = 2048
DEPTH = 4
DEC_BATCH = 8
DEC_SEQ = 4096
PAST_LEN = 128

N_MIXERS = 4
LN_EPS = 1e-5
DEEPNORM_ALPHA = (2 * DEPTH) ** 0.25
DEEPNORM_BETA = (8 * DEPTH) ** -0.25

CONV_WIDTH = 31

DIFF_HEADS = D_MODEL // 256
DIFF_HEAD_DIM = 128
DIFF_Q_BLOCK = 128
SUBLN_EPS = 1e-5

LRU_WIDTH = D_MODEL
LRU_BLOCK_W = 256
LRU_BLOCKS = LRU_WIDTH // LRU_BLOCK_W
LRU_CONV = 4
LRU_C = 8.0

SWA_HEADS = D_MODEL // 128
SWA_KV_HEADS = 4
SWA_HEAD_DIM = 128
SWA_WINDOW = 128
SWA_BLOCK = 128

FFN_DIM = 2 * D_MODEL
N_EXPERTS = 8
TOP_K = 2
EXPERT_DIM = D_MODEL // 2

kernel_name = 'hybrid_bidir_encoder_interleaved'

F32 = jnp.float32


def layer_norm(x, g, b):
    xf = x.astype(F32)
    mu = jnp.mean(xf, -1, keepdims=True)
    var = jnp.mean(jnp.square(xf - mu), -1, keepdims=True)
    y = (xf - mu) * lax.rsqrt(var + LN_EPS) * g.astype(F32) + b.astype(F32)
    return y.astype(x.dtype)


def alibi_slopes(n_heads):
    return 2.0 ** (-8.0 * jnp.arange(1, n_heads + 1, dtype=F32) / n_heads)


def depthwise_conv(x, w, b, pad_left, pad_right):
    y = lax.conv_general_dilated(
        x, w[:, None, :].astype(x.dtype), window_strides=(1,),
        padding=[(pad_left, pad_right)], dimension_numbers=('NWC', 'WIO', 'NWC'),
        feature_group_count=x.shape[-1])
    return y + b


def conformer_conv(x, w_in, b_in, w_dw, b_dw, norm_g, norm_b, w_out, b_out):
    h = x @ w_in + b_in
    a, g = jnp.split(h, 2, axis=-1)
    h = a * jax.nn.sigmoid(g)
    half = (CONV_WIDTH - 1) // 2
    h = depthwise_conv(h, w_dw, b_dw, half, half)
    h = jax.nn.silu(layer_norm(h, norm_g, norm_b))
    return h @ w_out + b_out


def diff_lambda_init(layer_idx):
    return 0.8 - 0.6 * math.exp(-0.3 * layer_idx)


def diff_attention(x, w_qkv, lam, subln_g, w_out, lambda_init):
    B, S, _ = x.shape
    H, d = DIFF_HEADS, DIFF_HEAD_DIM
    q, k, v = jnp.split(x @ w_qkv, 3, axis=-1)
    k = k.reshape(B, S, H, 2, d)
    v = v.reshape(B, S, H, 2 * d)
    nq = S // DIFF_Q_BLOCK
    qb = q.reshape(B, nq, DIFF_Q_BLOCK, H, 2, d).transpose(1, 0, 2, 3, 4, 5)
    lf = lam.astype(F32)
    lam_full = jnp.exp(jnp.sum(lf[0] * lf[1])) - jnp.exp(jnp.sum(lf[2] * lf[3])) + lambda_init
    slopes = alibi_slopes(H)[None, :, None, None, None]
    kpos = jnp.arange(S)
    scale = d ** -0.5

    def block(args):
        qi, j = args
        s = jnp.einsum('bqhcd,bkhcd->bhcqk', qi, k, preferred_element_type=F32) * scale
        qpos = j * DIFF_Q_BLOCK + jnp.arange(DIFF_Q_BLOCK)
        dist = jnp.abs(qpos[:, None] - kpos[None, :]).astype(F32)
        p = jax.nn.softmax(s - slopes * dist, axis=-1)
        attn = p[:, :, 0] - lam_full * p[:, :, 1]
        return jnp.einsum('bhqk,bkhe->bqhe', attn.astype(v.dtype), v)

    o = lax.map(block, (qb, jnp.arange(nq)))
    o = o.transpose(1, 0, 2, 3, 4).reshape(B, S, H, 2 * d).astype(F32)
    o = o * lax.rsqrt(jnp.mean(o * o, -1, keepdims=True) + SUBLN_EPS) * subln_g.astype(F32)
    o = (o * (1.0 - lambda_init)).reshape(B, S, H * 2 * d).astype(x.dtype)
    return o @ w_out


def rglru_direction(xf, gate_w, gate_b, lam, reverse):
    B, S, _ = xf.shape
    xb = xf.reshape(B, S, LRU_BLOCKS, LRU_BLOCK_W)
    gates = jnp.einsum('bsni,gnio->gbsno', xb, gate_w.astype(F32)).reshape(2, B, S, LRU_WIDTH)
    gates = gates + gate_b.astype(F32)[:, None, None, :]
    r = jax.nn.sigmoid(gates[0])
    i = jax.nn.sigmoid(gates[1])
    log_a = -LRU_C * r * jax.nn.softplus(-lam.astype(F32))
    a = jnp.exp(log_a)
    b = jnp.sqrt(-jnp.expm1(2.0 * log_a)) * (i * xf)

    def step(h, ab):
        a_t, b_t = ab
        h = a_t * h + b_t
        return h, h

    _, hs = lax.scan(step, jnp.zeros((B, LRU_WIDTH), F32),
                     (a.transpose(1, 0, 2), b.transpose(1, 0, 2)), reverse=reverse)
    return hs.transpose(1, 0, 2)


def recurrent_block(x, w_in, b_in, conv_w, conv_b, gate_w, gate_b, lam, w_out, b_out):
    h = x @ w_in + b_in
    y_branch, r_branch = jnp.split(h, 2, axis=-1)
    y_branch = jax.nn.gelu(y_branch)
    left = LRU_CONV // 2
    r_branch = depthwise_conv(r_branch, conv_w, conv_b, left, LRU_CONV - 1 - left)
    rf = r_branch.astype(F32)
    hr = (rglru_direction(rf, gate_w[0], gate_b[0], lam[0], False)
          + rglru_direction(rf, gate_w[1], gate_b[1], lam[1], True))
    return (hr.astype(x.dtype) * y_branch) @ w_out + b_out


def window_attention(x, w_qkv, sink, w_out):
    B, S, _ = x.shape
    H, KV, d = SWA_HEADS, SWA_KV_HEADS, SWA_HEAD_DIM
    G = H // KV
    nb = S // SWA_BLOCK
    qkv = x @ w_qkv
    q = qkv[..., :H * d].reshape(B, nb, SWA_BLOCK, KV, G, d)
    k = qkv[..., H * d:(H + KV) * d].reshape(B, S, KV, d)
    v = qkv[..., (H + KV) * d:].reshape(B, S, KV, d)
    pad = ((0, 0), (SWA_BLOCK, SWA_BLOCK), (0, 0), (0, 0))
    kp = jnp.pad(k, pad).reshape(B, nb + 2, SWA_BLOCK, KV, d)
    vp = jnp.pad(v, pad).reshape(B, nb + 2, SWA_BLOCK, KV, d)
    kw = jnp.concatenate([kp[:, :-2], kp[:, 1:-1], kp[:, 2:]], axis=2)
    vw = jnp.concatenate([vp[:, :-2], vp[:, 1:-1], vp[:, 2:]], axis=2)
    s = jnp.einsum('bnqhgd,bnchd->bnhgqc', q, kw, preferred_element_type=F32) * (d ** -0.5)
    blk = jnp.arange(nb)[:, None] * SWA_BLOCK
    qpos = blk + jnp.arange(SWA_BLOCK)[None, :]
    kpos = blk - SWA_BLOCK + jnp.arange(3 * SWA_BLOCK)[None, :]
    rel = jnp.abs(qpos[:, :, None] - kpos[:, None, :])
    valid = (rel <= SWA_WINDOW) & (kpos[:, None, :] >= 0) & (kpos[:, None, :] < S)
    slopes = alibi_slopes(H).reshape(KV, G)[None, None, :, :, None, None]
    s = s - slopes * rel.astype(F32)[None, :, None, None]
    s = jnp.where(valid[None, :, None, None], s, -jnp.inf)
    sink_b = sink.astype(F32).reshape(KV, G)[None, None, :, :, None, None]
    m = jnp.maximum(jnp.max(s, -1, keepdims=True), sink_b)
    p = jnp.exp(s - m)
    p = p / (jnp.sum(p, -1, keepdims=True) + jnp.exp(sink_b - m))
    o = jnp.einsum('bnhgqc,bnchd->bnqhgd', p.astype(vw.dtype), vw)
    return o.reshape(B, S, H * d) @ w_out


def swiglu(x, w_gate, w_up, w_down):
    return (jax.nn.silu(x @ w_gate) * (x @ w_up)) @ w_down


def moe_swiglu(x, w_router, w_gate, w_up, w_down):
    B, S, D = x.shape
    t = x.reshape(B * S, D)
    logits = (t @ w_router).astype(F32)
    top_v, top_i = lax.top_k(logits, TOP_K)
    top_w = jax.nn.softmax(top_v, axis=-1)
    gates = jnp.sum(jax.nn.one_hot(top_i, N_EXPERTS, dtype=F32) * top_w[..., None], axis=1)
    gates = gates.astype(t.dtype)
    g = jnp.einsum('td,edf->tef', t, w_gate)
    u = jnp.einsum('td,edf->tef', t, w_up)
    h = jax.nn.silu(g) * u * gates[:, :, None]
    out = jnp.einsum('tef,efd->td', h, w_down)
    return out.reshape(B, S, D)


def setup_inputs(seed: int = 0) -> dict:
    key = jax.random.key(seed)
    ks = iter(jax.random.split(key, 96))
    D = D_MODEL
    beta = DEEPNORM_BETA

    def w(shape, fan_in, scale=1.0):
        return jax.random.normal(next(ks), shape, F32) * (scale * fan_in ** -0.5)

    def gain(shape):
        return 1.0 + 0.02 * jax.random.normal(next(ks), shape, F32)

    def bias(shape):
        return 0.02 * jax.random.normal(next(ks), shape, F32)

    inp = {}
    inp['x_prompt'] = jax.random.normal(next(ks), (BATCH, SEQ, D), F32)
    inp['x_sample'] = jax.random.normal(next(ks), (DEC_BATCH, DEC_SEQ, D), F32)
    inp['l0_conv_w_in'] = w((D, 2 * D), D)
    inp['l0_conv_b_in'] = bias((2 * D,))
    inp['l0_conv_w_dw'] = w((CONV_WIDTH, D), CONV_WIDTH)
    inp['l0_conv_b_dw'] = bias((D,))
    inp['l0_conv_norm_g'] = gain((D,))
    inp['l0_conv_norm_b'] = bias((D,))
    inp['l0_conv_w_out'] = w((D, D), D, beta)
    inp['l0_conv_b_out'] = bias((D,))
    inp['l0_ln1_g'] = gain((D,))
    inp['l0_ln1_b'] = bias((D,))
    inp['l0_ffn_w_gate'] = w((D, FFN_DIM), D)
    inp['l0_ffn_w_up'] = w((D, FFN_DIM), D)
    inp['l0_ffn_w_down'] = w((FFN_DIM, D), FFN_DIM, beta)
    inp['l0_ln2_g'] = gain((D,))
    inp['l0_ln2_b'] = bias((D,))
    diff_w = DIFF_HEADS * 2 * DIFF_HEAD_DIM
    inp['l1_attn_w_qkv'] = w((D, 3 * diff_w), D)
    inp['l1_attn_lambda'] = 0.1 * jax.random.normal(next(ks), (4, DIFF_HEAD_DIM), F32)
    inp['l1_attn_subln_g'] = gain((2 * DIFF_HEAD_DIM,))
    inp['l1_attn_w_out'] = w((diff_w, D), diff_w, beta)
    inp['l1_ln1_g'] = gain((D,))
    inp['l1_ln1_b'] = bias((D,))
    inp['l1_moe_w_router'] = w((D, N_EXPERTS), D)
    inp['l1_moe_w_gate'] = w((N_EXPERTS, D, EXPERT_DIM), D)
    inp['l1_moe_w_up'] = w((N_EXPERTS, D, EXPERT_DIM), D)
    inp['l1_moe_w_down'] = w((N_EXPERTS, EXPERT_DIM, D), EXPERT_DIM, beta)
    inp['l1_ln2_g'] = gain((D,))
    inp['l1_ln2_b'] = bias((D,))
    inp['l2_rec_w_in'] = w((D, 2 * LRU_WIDTH), D)
    inp['l2_rec_b_in'] = bias((2 * LRU_WIDTH,))
    inp['l2_rec_conv_w'] = w((LRU_CONV, LRU_WIDTH), LRU_CONV)
    inp['l2_rec_conv_b'] = bias((LRU_WIDTH,))
    inp['l2_rec_gate_w'] = w((2, 2, LRU_BLOCKS, LRU_BLOCK_W, LRU_BLOCK_W), LRU_BLOCK_W)
    inp['l2_rec_gate_b'] = bias((2, 2, LRU_WIDTH))
    u = jax.random.uniform(next(ks), (2, LRU_WIDTH), F32, 0.9, 0.999)
    a_base = u ** (1.0 / LRU_C)
    inp['l2_rec_lambda'] = jnp.log(a_base) - jnp.log1p(-a_base)
    inp['l2_rec_w_out'] = w((LRU_WIDTH, D), LRU_WIDTH, beta)
    inp['l2_rec_b_out'] = bias((D,))
    inp['l2_ln1_g'] = gain((D,))
    inp['l2_ln1_b'] = bias((D,))
    inp['l2_ffn_w_gate'] = w((D, FFN_DIM), D)
    inp['l2_ffn_w_up'] = w((D, FFN_DIM), D)
    inp['l2_ffn_w_down'] = w((FFN_DIM, D), FFN_DIM, beta)
    inp['l2_ln2_g'] = gain((D,))
    inp['l2_ln2_b'] = bias((D,))
    swa_q = SWA_HEADS * SWA_HEAD_DIM
    inp['l3_attn_w_qkv'] = w((D, swa_q + 2 * SWA_KV_HEADS * SWA_HEAD_DIM), D)
    inp['l3_attn_sink'] = jax.random.normal(next(ks), (SWA_HEADS,), F32)
    inp['l3_attn_w_out'] = w((swa_q, D), swa_q, beta)
    inp['l3_ln1_g'] = gain((D,))
    inp['l3_ln1_b'] = bias((D,))
    inp['l3_moe_w_router'] = w((D, N_EXPERTS), D)
    inp['l3_moe_w_gate'] = w((N_EXPERTS, D, EXPERT_DIM), D)
    inp['l3_moe_w_up'] = w((N_EXPERTS, D, EXPERT_DIM), D)
    inp['l3_moe_w_down'] = w((N_EXPERTS, EXPERT_DIM, D), EXPERT_DIM, beta)
    inp['l3_ln2_g'] = gain((D,))
    inp['l3_ln2_b'] = bias((D,))
    return inp


def reference(x_prompt, x_sample,
              l0_conv_w_in, l0_conv_b_in, l0_conv_w_dw, l0_conv_b_dw, l0_conv_norm_g, l0_conv_norm_b,
              l0_conv_w_out, l0_conv_b_out, l0_ln1_g, l0_ln1_b, l0_ffn_w_gate, l0_ffn_w_up, l0_ffn_w_down,
              l0_ln2_g, l0_ln2_b,
              l1_attn_w_qkv, l1_attn_lambda, l1_attn_subln_g, l1_attn_w_out, l1_ln1_g, l1_ln1_b,
              l1_moe_w_router, l1_moe_w_gate, l1_moe_w_up, l1_moe_w_down, l1_ln2_g, l1_ln2_b,
              l2_rec_w_in, l2_rec_b_in, l2_rec_conv_w, l2_rec_conv_b, l2_rec_gate_w, l2_rec_gate_b,
              l2_rec_lambda, l2_rec_w_out, l2_rec_b_out, l2_ln1_g, l2_ln1_b, l2_ffn_w_gate, l2_ffn_w_up,
              l2_ffn_w_down, l2_ln2_g, l2_ln2_b,
              l3_attn_w_qkv, l3_attn_sink, l3_attn_w_out, l3_ln1_g, l3_ln1_b,
              l3_moe_w_router, l3_moe_w_gate, l3_moe_w_up, l3_moe_w_down, l3_ln2_g, l3_ln2_b):
    layers = [
        ((l0_conv_w_in, l0_conv_b_in, l0_conv_w_dw, l0_conv_b_dw, l0_conv_norm_g, l0_conv_norm_b,
          l0_conv_w_out, l0_conv_b_out), (l0_ln1_g, l0_ln1_b),
         (l0_ffn_w_gate, l0_ffn_w_up, l0_ffn_w_down), (l0_ln2_g, l0_ln2_b)),
        ((l1_attn_w_qkv, l1_attn_lambda, l1_attn_subln_g, l1_attn_w_out), (l1_ln1_g, l1_ln1_b),
         (l1_moe_w_router, l1_moe_w_gate, l1_moe_w_up, l1_moe_w_down), (l1_ln2_g, l1_ln2_b)),
        ((l2_rec_w_in, l2_rec_b_in, l2_rec_conv_w, l2_rec_conv_b, l2_rec_gate_w, l2_rec_gate_b,
          l2_rec_lambda, l2_rec_w_out, l2_rec_b_out), (l2_ln1_g, l2_ln1_b),
         (l2_ffn_w_gate, l2_ffn_w_up, l2_ffn_w_down), (l2_ln2_g, l2_ln2_b)),
        ((l3_attn_w_qkv, l3_attn_sink, l3_attn_w_out), (l3_ln1_g, l3_ln1_b),
         (l3_moe_w_router, l3_moe_w_gate, l3_moe_w_up, l3_moe_w_down), (l3_ln2_g, l3_ln2_b)),
    ]

    def trunk(x):
        for i in range(DEPTH):
            mixer_p, ln1, ffn_p, ln2 = layers[i]
            kind = i % N_MIXERS
            if kind == 0:
                h = conformer_conv(x, *mixer_p)
            elif kind == 1:
                h = diff_attention(x, *mixer_p, lambda_init=diff_lambda_init(i))
            elif kind == 2:
                h = recurrent_block(x, *mixer_p)
            else:
                h = window_attention(x, *mixer_p)
            x = layer_norm(DEEPNORM_ALPHA * x + h, *ln1)
            f = swiglu(x, *ffn_p) if i % 2 == 0 else moe_swiglu(x, *ffn_p)
            x = layer_norm(DEEPNORM_ALPHA * x + f, *ln2)
        return x

    y_prompt = trunk(x_prompt)
    y_sample = trunk(x_sample)
    return (y_prompt, y_sample)
```

```python
import math
from contextlib import ExitStack
import numpy as np
import concourse.bass as bass
import concourse.mybir as mybir
from concourse.bass_utils import run_bass_kernel_spmd

F32 = mybir.dt.float32
BF16 = mybir.dt.bfloat16
I32 = mybir.dt.int32
AF = mybir.ActivationFunctionType
ALU = mybir.AluOpType
AX = mybir.AxisListType

D = 2048
C = 16
TT = 512
ALPHA = 8.0 ** 0.25
LN_EPS = 1e-5
N_CORES = 8
import os
DBG = os.environ.get('KDBG', 'qnv2')

WSHAPES = {
    'l0_conv_w_in': (2048, 4096), 'l0_conv_b_in': (4096,), 'l0_conv_w_dw': (31, 2048), 'l0_conv_b_dw': (2048,),
    'l0_conv_norm_g': (2048,), 'l0_conv_norm_b': (2048,), 'l0_conv_w_out': (2048, 2048), 'l0_conv_b_out': (2048,),
    'l0_ln1_g': (2048,), 'l0_ln1_b': (2048,), 'l0_ffn_w_gate': (2048, 4096), 'l0_ffn_w_up': (2048, 4096),
    'l0_ffn_w_down': (4096, 2048), 'l0_ln2_g': (2048,), 'l0_ln2_b': (2048,),
    'l1_attn_w_qkv': (2048, 6144), 'l1_attn_lambda': (4, 128), 'l1_attn_subln_g': (256,), 'l1_attn_w_out': (2048, 2048),
    'l1_ln1_g': (2048,), 'l1_ln1_b': (2048,), 'l1_moe_w_router': (2048, 8), 'l1_moe_w_gate': (8, 2048, 1024),
    'l1_moe_w_up': (8, 2048, 1024), 'l1_moe_w_down': (8, 1024, 2048), 'l1_ln2_g': (2048,), 'l1_ln2_b': (2048,),
    'l2_rec_w_in': (2048, 4096), 'l2_rec_b_in': (4096,), 'l2_rec_conv_w': (4, 2048), 'l2_rec_conv_b': (2048,),
    'l2_rec_gate_w': (2, 2, 8, 256, 256), 'l2_rec_gate_b': (2, 2, 2048), 'l2_rec_lambda': (2, 2048),
    'l2_rec_w_out': (2048, 2048), 'l2_rec_b_out': (2048,), 'l2_ln1_g': (2048,), 'l2_ln1_b': (2048,),
    'l2_ffn_w_gate': (2048, 4096), 'l2_ffn_w_up': (2048, 4096), 'l2_ffn_w_down': (4096, 2048),
    'l2_ln2_g': (2048,), 'l2_ln2_b': (2048,),
    'l3_attn_w_qkv': (2048, 3072), 'l3_attn_sink': (16,), 'l3_attn_w_out': (2048, 2048),
    'l3_ln1_g': (2048,), 'l3_ln1_b': (2048,), 'l3_moe_w_router': (2048, 8), 'l3_moe_w_gate': (8, 2048, 1024),
    'l3_moe_w_up': (8, 2048, 1024), 'l3_moe_w_down': (8, 1024, 2048), 'l3_ln2_g': (2048,), 'l3_ln2_b': (2048,),
}
MATS = {
    'l0_conv_w_in': (2048, 4096, None, 2048, 256), 'l0_conv_w_out': (2048, 2048, None, 2048, 512),
    'l0_ffn_w_gate': (2048, 4096, None, 2048, 256), 'l0_ffn_w_up': (2048, 4096, None, 2048, 256),
    'l0_ffn_w_down': (4096, 2048, None, 4096, 256),
    'l1_attn_w_qkv': (2048, 6144, None, 2048, 512), 'l1_attn_w_out': (2048, 2048, None, 2048, 512),
    'l1_moe_w_gate': (16384, 1024, "e k n -> (e k) n", 2048, 256), 'l1_moe_w_up': (16384, 1024, "e k n -> (e k) n", 2048, 256),
    'l1_moe_w_down': (8192, 2048, "e k n -> (e k) n", 4096, 256),
    'l2_rec_w_in': (2048, 4096, None, 2048, 512), 'l2_rec_gate_w': (8192, 256, "a b n i o -> (a b n i) o", 2048, 256),
    'l2_rec_w_out': (2048, 2048, None, 2048, 256), 'l2_ffn_w_gate': (2048, 4096, None, 2048, 256),
    'l2_ffn_w_up': (2048, 4096, None, 2048, 256), 'l2_ffn_w_down': (4096, 2048, None, 4096, 256),
    'l3_attn_w_qkv': (2048, 3072, None, 2048, 512), 'l3_attn_w_out': (2048, 2048, None, 2048, 512),
    'l3_moe_w_gate': (16384, 1024, "e k n -> (e k) n", 2048, 256), 'l3_moe_w_up': (16384, 1024, "e k n -> (e k) n", 2048, 256),
    'l3_moe_w_down': (8192, 2048, "e k n -> (e k) n", 4096, 256),
}
VECS = ['l0_conv_b_in', 'l0_conv_w_dw', 'l0_conv_b_dw', 'l0_conv_norm_g', 'l0_conv_norm_b', 'l0_conv_b_out',
        'l0_ln1_g', 'l0_ln1_b', 'l0_ln2_g', 'l0_ln2_b', 'l1_ln1_g', 'l1_ln1_b', 'l1_ln2_g', 'l1_ln2_b',
        'l2_rec_b_in', 'l2_rec_conv_w', 'l2_rec_conv_b', 'l2_rec_gate_b', 'l2_rec_lambda', 'l2_rec_b_out',
        'l2_ln1_g', 'l2_ln1_b', 'l2_ln2_g', 'l2_ln2_b', 'l3_ln1_g', 'l3_ln1_b', 'l3_ln2_g', 'l3_ln2_b']


class Buf:
    __slots__ = ('t',)

    def __init__(self, t):
        self.t = t

    def __getitem__(self, idx):
        return self.t[idx]

    def get(self):
        return self


class Ring:
    def __init__(self, bufs):
        self.bufs = bufs
        self.i = 0

    def get(self):
        b = self.bufs[self.i % len(self.bufs)]
        self.i += 1
        return b


class Tr:
    ENG = ('pe', 'act', 'dve', 'pool', 'sp')

    def __init__(self, nc):
        self.nc = nc
        self.eng = dict(pe=nc.tensor, act=nc.scalar, dve=nc.vector, pool=nc.gpsimd, sp=nc.sync)
        self.esem = {e: (nc.alloc_semaphore("es_" + e), "es_" + e) for e in ('pe', 'act', 'dve', 'pool')}
        self.ecnt = {e: 0 for e in self.esem}
        self.waited = {e: {} for e in self.ENG}
        self.res = {}
        self.dsem = {'sp': [(nc.alloc_semaphore(f"dsp{i}"), f"dsp{i}") for i in range(20)],
                     'pool': [(nc.alloc_semaphore(f"dpl{i}"), f"dpl{i}") for i in range(12)]}
        self.dval = {}
        self.di = {'sp': 0, 'pool': 0}
        self.ninst = 0

    def _need(self, e, ev):
        sem, name, val, src = ev
        if src == 'pe' and e == 'pe':
            return
        if self.waited[e].get(name, 0) >= val:
            return
        self.eng[e].wait_ge(sem, val)
        self.waited[e][name] = val
        self.ninst += 1

    def _deps(self, e, reads, writes):
        for k in reads:
            r = self.res.get(k)
            if r is not None and r[0] is not None:
                self._need(e, r[0])
        for k in writes:
            r = self.res.get(k)
            if r is not None:
                if r[0] is not None:
                    self._need(e, r[0])
                for ev in r[1].values():
                    self._need(e, ev)

    def _record(self, ev, reads, writes):
        for k in reads:
            r = self.res.get(k)
            if r is None:
                r = self.res[k] = [None, {}]
            r[1][ev[1]] = ev
        for k in writes:
            self.res[k] = [ev, {}]

    def op(self, e, fn, reads=(), writes=()):
        self._deps(e, reads, writes)
        ins = fn(self.eng[e])
        self.ecnt[e] += 1
        sem, name = self.esem[e]
        ins.then_inc(sem, 1)
        self._record((sem, name, self.ecnt[e], e), reads, writes)
        self.ninst += 1

    def mm(self, items, reads, writes, transpose=False):
        self._deps('pe', reads, writes)
        pe = self.eng['pe']
        ins = None
        for it in items:
            if transpose:
                ins = pe.transpose(it[0], it[1], it[2])
            else:
                ins = pe.matmul(it[0], it[1], it[2], start=it[3], stop=it[4])
        self.ecnt['pe'] += 1
        sem, name = self.esem['pe']
        ins.then_inc(sem, 1)
        self._record((sem, name, self.ecnt['pe'], 'pe'), reads, writes)
        self.ninst += len(items)

    def dma(self, q, out, in_, reads=(), writes=()):
        sems = self.dsem[q]
        i = self.di[q]
        self.di[q] += 1
        sem, name = sems[i % len(sems)]
        prev = self.dval.get(name, 0)
        if prev:
            self._need(q, (sem, name, prev, 'dma'))
        self._deps(q, reads, writes)
        self.eng[q].dma_start(out=out, in_=in_).then_inc(sem, 16)
        self.dval[name] = prev + 16
        self._record((sem, name, prev + 16, 'dma'), reads, writes)
        self.ninst += 1

    def barrier(self, engines=None):
        evs = []
        for e, (sem, name) in self.esem.items():
            if self.ecnt[e]:
                evs.append((sem, name, self.ecnt[e], 'x'))
        for q in self.dsem:
            for sem, name in self.dsem[q]:
                v = self.dval.get(name, 0)
                if v:
                    evs.append((sem, name, v, 'dma'))
        for e in (engines or self.ENG):
            for ev in evs:
                self._need(e, ev)
        if engines is None:
            self.res = {}


def build(seq_lens, n_layers=4):
    NT = sum(seq_lens)
    SMAX = max(seq_lens)
    seq_starts = [sum(seq_lens[:i]) for i in range(len(seq_lens))]
    nseq = len(seq_lens)
    nc = bass.Bass("TRN2", target_bir_lowering=False)
    tr = Tr(nc)
    gstack = ExitStack()

    X = nc.dram_tensor("x", [NT, D], F32, kind="ExternalInput").ap()
    Y = nc.dram_tensor("y", [NT, D], F32, kind="ExternalOutput").ap()
    WIN = {n: nc.dram_tensor(n, list(s), F32, kind="ExternalInput").ap() for n, s in WSHAPES.items()}

    def scr(name, shape, dt):
        return nc.dram_tensor(name, list(shape), dt, kind="Internal").ap()

    WB = {n: scr(n + "_bf", [r // rg, c // sw, 128, (rg // 128) * sw], BF16) for n, (r, c, _, rg, sw) in MATS.items()}
    XB = scr("XB", [D, NT], BF16)
    XB1 = scr("XB1", [D, NT], BF16)
    GLU = scr("GLU", [D, NT], BF16)
    QKT = scr("QKT", [4096, NT], BF16)
    VV = scr("VV", [NT, 2048], BF16)
    OT = scr("OT", [D, NT], BF16)
    YB = scr("YB", [D, NT], BF16)
    RB = scr("RB", [D, NT], F32)
    HF = scr("HF", [D, NT], F32)
    Q3 = scr("Q3", [D, NT], BF16)
    K3 = scr("K3", [512, NT], BF16)
    V3 = scr("V3", [NT, 512], BF16)
    DIAG = scr("DIAG", [C, 128, 31 * 128], BF16)

    uid = [0]

    def sb(es, name, shape, dt, n=1):
        uid[0] += 1
        bufs = [Buf(es.enter_context(nc.sbuf_tensor(f"{name}_{uid[0]}_{i}", list(shape), dt))) for i in range(n)]
        return bufs[0] if n == 1 else Ring(bufs)

    PS = Ring([Buf(gstack.enter_context(nc.psum_tensor(f"ps{i}", [128, 512], F32))) for i in range(8)])
    nvec = sum(int(np.prod(WSHAPES[n])) // 2048 for n in VECS)
    VEC = sb(gstack, "vec", [128, nvec, 16], F32)
    vidx = {}
    _o = 0
    for n in VECS:
        vidx[n] = _o
        _o += int(np.prod(WSHAPES[n])) // 2048
    IDF = sb(gstack, "idf", [128, 128], F32)
    IDB = sb(gstack, "idb", [128, 128], BF16)
    ONESM = sb(gstack, "onesm", [128, 128], BF16)
    ONES1 = sb(gstack, "ones1", [128, 128], BF16)
    CHALF = sb(gstack, "chalf", [128, 512], F32)
    CNHALF = sb(gstack, "cnhalf", [128, 512], F32)
    NRM = sb(gstack, "nrm", [128, nseq, 32], F32)
    NRM3 = sb(gstack, "nrm3", [128, nseq, 20], F32)
    NSP = sb(gstack, "nsp", [128, 2, 16], F32)
    NGB = sb(gstack, "ngb", [128, 4, 16], F32)
    LAMC = sb(gstack, "lamc", [128, 1], F32)
    GROW = sb(gstack, "grow", [128, 256], F32)
    SINK = sb(gstack, "sink", [128, 16], F32)
    SEL = sb(gstack, "sel", [8, 8, 128], F32)
    WR = {1: sb(gstack, "wr1", [128, 16, 8], BF16), 3: sb(gstack, "wr3", [128, 16, 8], BF16)}

    def vcol(name, j, c):
        return VEC[:, vidx[name] + j, c:c + 1]

    def setup():
        with ExitStack() as es:
            ti = sb(es, "s_ti", [128, 512], I32)
            tf = sb(es, "s_tf", [128, 512], F32)
            tr.op('pool', lambda g: g.iota(ti[:, 0:128], pattern=[[-1, 128]], base=0, channel_multiplier=1), writes=[ti])
            tr.op('dve', lambda v: v.tensor_copy(out=tf[:, 0:128], in_=ti[:, 0:128]), reads=[ti], writes=[tf])
            tr.op('dve', lambda v: v.tensor_single_scalar(out=IDF[:, :], in_=tf[:, 0:128], scalar=0.0, op=ALU.is_equal),
                  reads=[tf], writes=[IDF])
            tr.op('dve', lambda v: v.tensor_copy(out=IDB[:, :], in_=IDF[:, :]), reads=[IDF], writes=[IDB])
            tr.op('pool', lambda g: g.memset(ONESM[:, :], 1.0 / 2048.0), writes=[ONESM])
            tr.op('pool', lambda g: g.memset(ONES1[:, :], 1.0), writes=[ONES1])
            tr.op('pool', lambda g: g.memset(CHALF[:, :], 0.5), writes=[CHALF])
            tr.op('pool', lambda g: g.memset(CNHALF[:, :], -0.5), writes=[CNHALF])
            tr.op('pool', lambda g: g.memset(NRM[:, :, :], 0.0), writes=[NRM])
            tr.op('pool', lambda g: g.memset(NRM3[:, :, :], 0.0), writes=[NRM3])
            ti2 = sb(es, "s_ti2", [8, 8, 128], I32)
            tf2 = sb(es, "s_tf2", [8, 8, 128], F32)
            tr.op('pool', lambda g: g.iota(ti2[:, :, :], pattern=[[-1, 8], [0, 128]], base=0, channel_multiplier=1), writes=[ti2])
            tr.op('dve', lambda v: v.tensor_copy(out=tf2[:, :, :], in_=ti2[:, :, :]), reads=[ti2], writes=[tf2])
            tr.op('dve', lambda v: v.tensor_single_scalar(out=SEL[:, :, :], in_=tf2[:, :, :], scalar=0.0, op=ALU.is_equal),
                  reads=[tf2], writes=[SEL])
            stg = sb(es, "s_stg", [16, nvec, 128], F32)
            for n in VECS:
                k = int(np.prod(WSHAPES[n])) // 2048
                src = WIN[n]
                nd = len(WSHAPES[n])
                if nd == 1:
                    v2 = src.rearrange("(j c p) -> c j p", c=16, p=128)
                elif nd == 2:
                    v2 = src.rearrange("j (c p) -> c j p", p=128)
                else:
                    v2 = src.rearrange("a b (c p) -> c (a b) p", p=128)
                tr.dma('sp', stg[:, vidx[n]:vidx[n] + k, :], v2, writes=[stg])
            for v0 in range(0, nvec, 32):
                nv = min(32, nvec - v0)
                ps = PS.get()
                tr.mm([(ps[:, j * 16:(j + 1) * 16], stg[:, v0 + j, :], IDF[0:16, 0:16]) for j in range(nv)],
                      reads=[stg, IDF], writes=[ps], transpose=True)
                tr.op('dve', lambda v, ps=ps, v0=v0, nv=nv: v.tensor_copy(
                    out=VEC[:, v0:v0 + nv, :], in_=ps[:, 0:nv * 16].rearrange("p (j c) -> p j c", c=16)),
                    reads=[ps], writes=[VEC])
            for L in (1, 3):
                rt = sb(es, f"s_rt{L}", [128, 16, 8], F32)
                tr.dma('sp', rt[:, :, :], WIN[f'l{L}_moe_w_router'].rearrange("(c p) e -> p c e", p=128), writes=[rt])
                tr.op('dve', lambda v, rt=rt, L=L: v.tensor_copy(out=WR[L][:, :, :], in_=rt[:, :, :]), reads=[rt], writes=[WR[L]])
            tr.dma('sp', SINK[:, :], WIN['l3_attn_sink'].partition_broadcast(128), writes=[SINK])
            tr.dma('sp', GROW[:, :], WIN['l1_attn_subln_g'].partition_broadcast(128), writes=[GROW])
            lam_init = 0.8 - 0.6 * math.exp(-0.3 * 1)
            tr.op('dve', lambda v: v.tensor_scalar(out=GROW[:, :], in0=GROW[:, :], scalar1=1.0 - lam_init, scalar2=None,
                                                   op0=ALU.mult), reads=[GROW], writes=[GROW])
            lt = sb(es, "s_lt", [128, 512], F32)
            tr.dma('sp', lt[:, :], WIN['l1_attn_lambda'].rearrange("a d -> (a d)").partition_broadcast(128), writes=[lt])
            pr = sb(es, "s_pr", [128, 256], F32)
            tr.op('dve', lambda v: v.tensor_tensor(out=pr[:, 0:128], in0=lt[:, 0:128], in1=lt[:, 128:256], op=ALU.mult),
                  reads=[lt], writes=[pr])
            tr.op('dve', lambda v: v.tensor_tensor(out=pr[:, 128:256], in0=lt[:, 256:384], in1=lt[:, 384:512], op=ALU.mult),
                  reads=[lt, pr], writes=[pr])
            sm = sb(es, "s_sm", [128, 4], F32)
            tr.op('dve', lambda v: v.tensor_reduce(out=sm[:, 0:2], in_=pr[:, :].rearrange("p (a d) -> p a d", a=2),
                                                   axis=AX.X, op=ALU.add), reads=[pr], writes=[sm])
            tr.op('act', lambda a: a.activation(out=sm[:, 2:4], in_=sm[:, 0:2], func=AF.Exp), reads=[sm], writes=[sm])
            tr.op('dve', lambda v: v.tensor_tensor(out=LAMC[:, :], in0=sm[:, 2:3], in1=sm[:, 3:4], op=ALU.subtract),
                  reads=[sm], writes=[LAMC])
            tr.op('dve', lambda v: v.tensor_scalar(out=LAMC[:, :], in0=LAMC[:, :], scalar1=lam_init, scalar2=None, op0=ALU.add),
                  reads=[LAMC], writes=[LAMC])
            li = vidx['l2_rec_lambda']
            ex = sb(es, "s_ex", [128, 2, 16], F32)
            l1 = sb(es, "s_l1", [128, 2, 16], F32)
            l2 = sb(es, "s_l2", [128, 2, 16], F32)
            mk = sb(es, "s_mk", [128, 2, 16], F32)
            tr.op('act', lambda a: a.activation(out=ex[:, :, :], in_=VEC[:, li:li + 2, :], func=AF.Exp, scale=-1.0),
                  reads=[VEC], writes=[ex])
            tr.op('act', lambda a: a.activation(out=l1[:, :, :], in_=ex[:, :, :], func=AF.Ln, bias=1.0), reads=[ex], writes=[l1])
            tr.op('dve', lambda v: v.tensor_scalar(out=l2[:, :, :], in0=ex[:, :, :], scalar1=-0.2, scalar2=0.25, op0=ALU.mult, op1=ALU.add),
                  reads=[ex], writes=[l2])
            for cst in (1.0 / 3.0, 0.5, 1.0):
                tr.op('dve', lambda v: v.tensor_tensor(out=l2[:, :, :], in0=l2[:, :, :], in1=ex[:, :, :], op=ALU.mult),
                      reads=[l2, ex], writes=[l2])
                tr.op('dve', lambda v, cst=cst: v.tensor_scalar(out=l2[:, :, :], in0=l2[:, :, :], scalar1=-1.0, scalar2=cst,
                                                                 op0=ALU.mult, op1=ALU.add), reads=[l2], writes=[l2])
            tr.op('dve', lambda v: v.tensor_tensor(out=l2[:, :, :], in0=l2[:, :, :], in1=ex[:, :, :], op=ALU.mult),
                  reads=[l2, ex], writes=[l2])
            tr.op('dve', lambda v: v.tensor_single_scalar(out=mk[:, :, :], in_=ex[:, :, :], scalar=0.25, op=ALU.is_gt),
                  reads=[ex], writes=[mk])
            tr.op('dve', lambda v: v.tensor_tensor(out=l1[:, :, :], in0=l1[:, :, :], in1=l2[:, :, :], op=ALU.subtract),
                  reads=[l1, l2], writes=[l1])
            tr.op('dve', lambda v: v.tensor_tensor(out=l1[:, :, :], in0=l1[:, :, :], in1=mk[:, :, :], op=ALU.mult),
                  reads=[l1, mk], writes=[l1])
            tr.op('dve', lambda v: v.tensor_tensor(out=l1[:, :, :], in0=l1[:, :, :], in1=l2[:, :, :], op=ALU.add),
                  reads=[l1, l2], writes=[l1])
            tr.op('dve', lambda v: v.tensor_scalar(out=NSP[:, :, :], in0=l1[:, :, :], scalar1=-8.0, scalar2=None, op0=ALU.mult),
                  reads=[l1], writes=[NSP])
            gbi = vidx['l2_rec_gate_b']
            tr.op('dve', lambda v: v.tensor_scalar(out=NGB[:, :, :], in0=VEC[:, gbi:gbi + 4, :], scalar1=-1.0, scalar2=None, op0=ALU.mult),
                  reads=[VEC], writes=[NGB])
            tr.barrier()

    def convert(names):
        with ExitStack() as es:
            fr = sb(es, "cv_f", [128, 4096], F32, 3)
            br = sb(es, "cv_b", [128, 4096], BF16, 3)
            i = 0
            for n in names:
                R, N, rs, RG, sw = MATS[n]
                src = WIN[n] if rs is None else WIN[n].rearrange(rs)
                dst = WB[n]
                for rb in range(R // 128):
                    g_ = (rb * 128) // RG
                    k_ = ((rb * 128) % RG) // 128
                    for n0 in range(0, N, 4096):
                        nn = min(4096, N - n0)
                        fb = fr.get()
                        bb = br.get()
                        tr.dma('sp', fb[:, 0:nn], src[rb * 128:(rb + 1) * 128, n0:n0 + nn], writes=[fb])
                        if i % 2 == 0:
                            tr.op('dve', lambda v, fb=fb, bb=bb, m=nn: v.tensor_copy(out=bb[:, 0:m], in_=fb[:, 0:m]),
                                  reads=[fb], writes=[bb])
                        else:
                            tr.op('act', lambda a, fb=fb, bb=bb, m=nn: a.copy(out=bb[:, 0:m], in_=fb[:, 0:m]),
                                  reads=[fb], writes=[bb])
                        i += 1
                        dview = dst[g_, n0 // sw:(n0 + nn) // sw, :, k_ * sw:(k_ + 1) * sw].rearrange("s p n -> p s n")
                        tr.dma('pool', dview, bb[:, 0:nn].rearrange("p (s n) -> p s n", n=sw), reads=[bb], writes=[('W', n)])
            tr.barrier()

    def linear(wring, KC, groups, rhs_fn, rhs_reads, epi, order=None, W=TT):
        for gi, grp in enumerate(groups):
            slab = wring.get()
            o = 0
            srcs = []
            for a in grp:
                w = a.shape[1]
                tr.dma('sp', slab[:, o:o + w], a, writes=[slab])
                srcs.append((o, w // KC))
                o += w
            j = 0
            for (o, sw) in srcs:
                for jl in range(sw // 128):
                    ps = PS.get()
                    tr.mm([(ps[:, 0:W], slab[:, o + k * sw + jl * 128:o + k * sw + (jl + 1) * 128], rhs_fn(k), k == 0, k == KC - 1)
                           for k in range(KC)], reads=[slab] + rhs_reads, writes=[ps])
                    epi(gi, j, ps)
                    j += 1

    def layernorm(es_bufs, S, gname, bname, out, silu=False, W=TT, eps=LN_EPS):
        sq, xb, mean_sb, t1, t2 = es_bufs
        mps = PS.get()
        eps_ = PS.get()
        for c in range(C):
            q_ = sq.get()
            x_ = xb.get()
            tr.op('act', lambda a, c=c, q_=q_: a.activation(out=q_[:, 0:W], in_=S[:, c, 0:W], func=AF.Square), reads=[S], writes=[q_])
            tr.op('pool', lambda g, c=c, x_=x_: g.tensor_copy(out=x_[:, 0:W], in_=S[:, c, 0:W]), reads=[S], writes=[x_])
            tr.mm([(mps[:, 0:W], ONESM[:, :], x_[:, 0:W], c == 0, c == C - 1)], reads=[x_, ONESM], writes=[mps])
            tr.mm([(eps_[:, 0:W], ONESM[:, :], q_[:, 0:W], c == 0, c == C - 1)], reads=[q_, ONESM], writes=[eps_])
        tr.op('act', lambda a: a.copy(out=mean_sb[:, 0:W], in_=mps[:, 0:W]), reads=[mps], writes=[mean_sb])
        msq = t1.get()
        tr.op('dve', lambda v: v.tensor_tensor(out=msq[:, 0:W], in0=mean_sb[:, 0:W], in1=mean_sb[:, 0:W], op=ALU.mult),
              reads=[mean_sb], writes=[msq])
        var = t1.get()
        tr.op('dve', lambda v: v.scalar_tensor_tensor(out=var[:, 0:W], in0=eps_[:, 0:W], scalar=eps, in1=msq[:, 0:W],
                                                      op0=ALU.add, op1=ALU.subtract), reads=[eps_, msq], writes=[var])
        rstd = t1.get()
        tr.op('act', lambda a: a.activation(out=rstd[:, 0:W], in_=var[:, 0:W], func=AF.Sqrt), reads=[var], writes=[rstd])
        tr.op('dve', lambda v: v.reciprocal(out=rstd[:, 0:W], in_=rstd[:, 0:W]), reads=[rstd], writes=[rstd])
        gi, bi = vidx[gname], vidx[bname]
        for c in range(C):
            a_ = t2.get()
            tr.op('pool', lambda g, c=c, a_=a_: g.tensor_tensor(out=a_[:, 0:W], in0=S[:, c, 0:W], in1=mean_sb[:, 0:W], op=ALU.subtract),
                  reads=[S, mean_sb], writes=[a_])
            tr.op('dve', lambda v, a_=a_: v.tensor_tensor(out=a_[:, 0:W], in0=a_[:, 0:W], in1=rstd[:, 0:W], op=ALU.mult),
                  reads=[a_, rstd], writes=[a_])
            tr.op('act', lambda a, c=c, a_=a_: a.activation(out=out[:, c, 0:W], in_=a_[:, 0:W], func=(AF.Silu if silu else AF.Identity),
                                                           bias=VEC[:, bi, c:c + 1], scale=VEC[:, gi, c:c + 1]),
                  reads=[a_, VEC], writes=[out])

    def ln_bufs(es, W=TT):
        return (sb(es, "ln_sq", [128, W], BF16, 3), sb(es, "ln_xb", [128, W], BF16, 3), sb(es, "ln_mean", [128, W], F32),
                sb(es, "ln_t1", [128, W], F32, 3), sb(es, "ln_t2", [128, W], F32, 4))

    def fm_view(dram, t0, w, rows0=0, nch=C):
        return dram[rows0:rows0 + nch * 128, t0:t0 + w].rearrange("(c p) t -> p c t", p=128)

    tiles = []
    for si in range(nseq):
        for k in range(seq_lens[si] // TT):
            tiles.append((seq_starts[si] + k * TT, si, seq_starts[si], seq_lens[si]))

    def stage_l0a():
        with ExitStack() as es:
            xin = sb(es, "a_xin", [128, 4, D], F32, 2)
            xb = sb(es, "a_xb", [128, C, TT], BF16, 2)
            gl = sb(es, "a_gl", [128, C, TT], BF16, 2)
            wr = sb(es, "a_w", [128, 8192], BF16, 2)
            sg = sb(es, "a_sg", [128, TT], F32, 4)
            bi = vidx['l0_conv_b_in']
            for (t0, si, s0, sl) in tiles:
                xi = xin.get()
                tr.dma('sp', xi[:, :, :], X[t0:t0 + TT, :].rearrange("(b p) d -> p b d", p=128), writes=[xi])
                xbt = xb.get()
                for c in range(C):
                    ps = PS.get()
                    tr.mm([(ps[:, b * 128:(b + 1) * 128], xi[:, b, c * 128:(c + 1) * 128], IDF[:, :]) for b in range(4)],
                          reads=[xi, IDF], writes=[ps], transpose=True)
                    if c % 2 == 0:
                        tr.op('dve', lambda v, ps=ps, c=c: v.tensor_copy(out=xbt[:, c, :], in_=ps[:, :]), reads=[ps], writes=[xbt])
                    else:
                        tr.op('act', lambda a, ps=ps, c=c: a.copy(out=xbt[:, c, :], in_=ps[:, :]), reads=[ps], writes=[xbt])
                tr.dma('pool', fm_view(XB, t0, TT), xbt[:, :, :], reads=[xbt], writes=[('XB', t0)])
                glt = gl.get()
                wb = WB['l0_conv_w_in']
                groups = [[wb[0, c0 // 2], wb[0, 8 + c0 // 2]] for c0 in range(0, C, 2)]
                hold = {}

                def epi(gi, j, ps):
                    if j < 2:
                        hold[j] = ps
                        return
                    c = 2 * gi + (j - 2)
                    pa = hold.pop(j - 2)
                    s_ = sg.get()
                    tr.op('act', lambda a: a.activation(out=s_[:, :], in_=ps[:, :], func=AF.Sigmoid, bias=VEC[:, bi + 1, c:c + 1]),
                          reads=[ps, VEC], writes=[s_])
                    tr.op('dve', lambda v: v.scalar_tensor_tensor(out=glt[:, c, :], in0=pa[:, :], scalar=VEC[:, bi, c:c + 1], in1=s_[:, :],
                                                                  op0=ALU.add, op1=ALU.mult), reads=[pa, s_, VEC], writes=[glt])
                linear(wr, C, groups, lambda k: xbt[:, k, :], [xbt], epi)
                tr.dma('pool', fm_view(GLU, t0, TT), glt[:, :, :], reads=[glt], writes=[('GLU', t0)])
            tr.barrier()

    def resid_ln(es, name):
        return None

    def stage_l0b():
        HALO = 15
        with ExitStack() as es:
            G = sb(es, "b_g", [128, C, TT + 2 * HALO], BF16, 1)
            S = sb(es, "b_s", [128, C, TT], F32, 2)
            A = sb(es, "b_a", [128, C, TT], BF16, 2)
            wr = sb(es, "b_w", [128, 8192], BF16, 2)
            DGR = sb(es, "b_dg", [128, 31 * 128], BF16, 2)
            tmp = sb(es, "b_tmp", [128, TT], F32, 4)
            lnb = ln_bufs(es)
            wi = vidx['l0_conv_w_dw']
            bdw = vidx['l0_conv_b_dw']
            bo = vidx['l0_conv_b_out']
            for c in range(C):
                dg = DGR.get()
                for k in range(31):
                    if k % 2 == 0:
                        tr.op('dve', lambda v, dg=dg, k=k, c=c: v.tensor_scalar(out=dg[:, k * 128:(k + 1) * 128], in0=IDB[:, :],
                                                                                scalar1=VEC[:, wi + k, c:c + 1], scalar2=None, op0=ALU.mult),
                              reads=[IDB, VEC], writes=[dg])
                    else:
                        tr.op('act', lambda a, dg=dg, k=k, c=c: a.activation(out=dg[:, k * 128:(k + 1) * 128], in_=IDB[:, :], func=AF.Identity,
                                                                             scale=VEC[:, wi + k, c:c + 1]), reads=[IDB, VEC], writes=[dg])
                tr.dma('pool', DIAG[c], dg[:, :], reads=[dg], writes=[('DIAG', c)])
            tr.barrier()
            for (t0, si, s0, sl) in tiles:
                g = G
                lo = max(t0 - HALO, s0)
                hi = min(t0 + TT + HALO, s0 + sl)
                if lo > t0 - HALO:
                    tr.op('pool', lambda p_, g=g: p_.memset(g[:, :, 0:HALO], 0.0), writes=[g])
                if hi < t0 + TT + HALO:
                    tr.op('pool', lambda p_, g=g: p_.memset(g[:, :, TT + HALO:TT + 2 * HALO], 0.0), writes=[g])
                off = lo - (t0 - HALO)
                tr.dma('sp', g[:, :, off:off + (hi - lo)], fm_view(GLU, lo, hi - lo),
                       reads=[('GLU', t) for t in range((lo // TT) * TT, hi, TT)], writes=[g])
                xres = A.get()
                tr.dma('sp', xres[:, :, :], fm_view(XB, t0, TT), reads=[('XB', t0)], writes=[xres])
                s1 = S.get()
                for c in range(C):
                    dg = DGR.get()
                    tr.dma('sp', dg[:, :], DIAG[c], writes=[dg])
                    ps = PS.get()
                    tr.mm([(ps[:, :], dg[:, k * 128:(k + 1) * 128], g[:, c, k:k + TT], k == 0, k == 30) for k in range(31)],
                          reads=[dg, g], writes=[ps])
                    tr.op('act', lambda a, ps=ps, c=c: a.activation(out=s1[:, c, :], in_=ps[:, :], func=AF.Identity, bias=VEC[:, bdw, c:c + 1]),
                          reads=[ps, VEC], writes=[s1])
                hA = A.get()
                layernorm(lnb, s1, 'l0_conv_norm_g', 'l0_conv_norm_b', hA, silu=True)
                s2 = S.get()
                wb = WB['l0_conv_w_out']
                groups = [[wb[0, n0 // 512]] for n0 in range(0, D, 512)]

                def epi(gi, j, ps):
                    c = gi * 4 + j
                    t_ = tmp.get()
                    tr.op('act', lambda a: a.activation(out=t_[:, :], in_=ps[:, :], func=AF.Identity, bias=VEC[:, bo, c:c + 1]),
                          reads=[ps, VEC], writes=[t_])
                    tr.op('dve', lambda v: v.scalar_tensor_tensor(out=s2[:, c, :], in0=xres[:, c, :], scalar=ALPHA, in1=t_[:, :],
                                                                  op0=ALU.mult, op1=ALU.add), reads=[xres, t_], writes=[s2])
                linear(wr, C, groups, lambda k: hA[:, k, :], [hA], epi)
                x1 = A.get()
                layernorm(lnb, s2, 'l0_ln1_g', 'l0_ln1_b', x1)
                tr.dma('pool', fm_view(XB1, t0, TT), x1[:, :, :], reads=[x1], writes=[('XB1', t0)])
            tr.barrier()

    def tail_l1_qkv(es, wr=None):
        oc = sb(es, "t_oc", [128, TT], BF16, 4)
        sqt = sb(es, "t_sq", [128, TT], BF16, 3)
        mx = sb(es, "t_mx", [128, 8], F32, 4)
        vt = sb(es, "t_vt", [128, 512], BF16, 4)
        wb = WB['l1_attn_w_qkv']

        def run(x2, t0, si):
            groups = [[wb[0, n0 // 512]] for n0 in range(0, 4096, 512)]

            def epi(gi, j, ps):
                ch = gi * 4 + j
                o_ = oc.get()
                tr.op('dve', lambda v: v.tensor_copy(out=o_[:, :], in_=ps[:, :]), reads=[ps], writes=[o_])
                tr.dma('pool', QKT[ch * 128:(ch + 1) * 128, t0:t0 + TT], o_[:, :], reads=[o_], writes=[('QKT', t0)])
                if 'n' not in DBG:
                    return
                s_ = sqt.get()
                tr.op('act', lambda a: a.activation(out=s_[:, :], in_=o_[:, :], func=AF.Square), reads=[o_], writes=[s_])
                p2 = PS.get()
                tr.mm([(p2[:, :], ONES1[:, :], s_[:, :], True, True)], reads=[s_, ONES1], writes=[p2])
                m_ = mx.get()
                tr.op('dve', lambda v: v.max(out=m_[:, :], in_=p2[:, :]), reads=[p2], writes=[m_])
                if '2' not in DBG:
                    return
                tr.op('dve', lambda v: v.tensor_tensor(out=NRM[:, si, ch:ch + 1], in0=NRM[:, si, ch:ch + 1], in1=m_[:, 0:1], op=ALU.max),
                      reads=[m_, NRM], writes=[NRM])
            if 'q' in DBG:
                linear(wr, C, groups, lambda k: x2[:, k, :], [x2], epi)
            for n0 in (range(0, 2048, 512) if 'v' in DBG else []):
                slab = wr.get()
                sv = slab[:, 0:C * 512].rearrange("p (k n) -> p k n", k=C)
                tr.dma('sp', slab[:, 0:C * 512], wb[0, 8 + n0 // 512], writes=[slab])
                for b in range(4):
                    ps = PS.get()
                    tr.mm([(ps[:, :], x2[:, k, b * 128:(b + 1) * 128], sv[:, k, :], k == 0, k == C - 1) for k in range(C)],
                          reads=[slab, x2], writes=[ps])
                    v_ = vt.get()
                    tr.op('act', lambda a, ps=ps, v_=v_: a.copy(out=v_[:, :], in_=ps[:, :]), reads=[ps], writes=[v_])
                    tr.dma('pool', VV[t0 + b * 128:t0 + (b + 1) * 128, n0:n0 + 512], v_[:, :], reads=[v_], writes=[('VV', t0)])
        return run

    def tail_l2_win(es, wr=None):
        ob = sb(es, "t_ob", [128, TT], BF16, 4)
        of = sb(es, "t_of", [128, TT], F32, 4)
        wb = WB['l2_rec_w_in']
        bi = vidx['l2_rec_b_in']

        def run(x2, t0, si):
            groups = [[wb[0, n0 // 512]] for n0 in range(0, 4096, 512)]

            def epi(gi, j, ps):
                ch = gi * 4 + j
                if ch < 16:
                    o_ = ob.get()
                    tr.op('act', lambda a: a.activation(out=o_[:, :], in_=ps[:, :], func=AF.Gelu_apprx_tanh, bias=VEC[:, bi, ch:ch + 1]),
                          reads=[ps, VEC], writes=[o_])
                    tr.dma('pool', YB[ch * 128:(ch + 1) * 128, t0:t0 + TT], o_[:, :], reads=[o_], writes=[('YB', t0)])
                else:
                    c = ch - 16
                    o_ = of.get()
                    tr.op('dve', lambda v: v.tensor_scalar(out=o_[:, :], in0=ps[:, :], scalar1=VEC[:, bi + 1, c:c + 1], scalar2=None, op0=ALU.add),
                          reads=[ps, VEC], writes=[o_])
                    tr.dma('pool', RB[c * 128:(c + 1) * 128, t0:t0 + TT], o_[:, :], reads=[o_], writes=[('RB', t0)])
            linear(wr, C, groups, lambda k: x2[:, k, :], [x2], epi)
        return run

    def tail_l3_qkv(es, wr=None):
        oc = sb(es, "t_oc", [128, TT], BF16, 4)
        sqt = sb(es, "t_sq", [128, TT], BF16, 3)
        mx = sb(es, "t_mx", [128, 8], F32, 4)
        vt = sb(es, "t_vt", [128, 512], BF16, 4)
        wb = WB['l3_attn_w_qkv']

        def run(x2, t0, si):
            groups = [[wb[0, n0 // 512]] for n0 in range(0, 2560, 512)]

            def epi(gi, j, ps):
                ch = gi * 4 + j
                o_ = oc.get()
                tr.op('dve', lambda v: v.tensor_copy(out=o_[:, :], in_=ps[:, :]), reads=[ps], writes=[o_])
                if ch < 16:
                    tr.dma('pool', Q3[ch * 128:(ch + 1) * 128, t0:t0 + TT], o_[:, :], reads=[o_], writes=[('Q3', t0)])
                else:
                    tr.dma('pool', K3[(ch - 16) * 128:(ch - 15) * 128, t0:t0 + TT], o_[:, :], reads=[o_], writes=[('K3', t0)])
                s_ = sqt.get()
                tr.op('act', lambda a: a.activation(out=s_[:, :], in_=o_[:, :], func=AF.Square), reads=[o_], writes=[s_])
                p2 = PS.get()
                tr.mm([(p2[:, :], ONES1[:, :], s_[:, :], True, True)], reads=[s_, ONES1], writes=[p2])
                m_ = mx.get()
                tr.op('dve', lambda v: v.max(out=m_[:, :], in_=p2[:, :]), reads=[p2], writes=[m_])
                tr.op('dve', lambda v: v.tensor_tensor(out=NRM3[:, si, ch:ch + 1], in0=NRM3[:, si, ch:ch + 1], in1=m_[:, 0:1], op=ALU.max),
                      reads=[m_, NRM3], writes=[NRM3])
            linear(wr, C, groups, lambda k: x2[:, k, :], [x2], epi)
            slab = wr.get()
            sv = slab[:, 0:C * 512].rearrange("p (k n) -> p k n", k=C)
            tr.dma('sp', slab[:, 0:C * 512], wb[0, 5], writes=[slab])
            for b in range(4):
                ps = PS.get()
                tr.mm([(ps[:, :], x2[:, k, b * 128:(b + 1) * 128], sv[:, k, :], k == 0, k == C - 1) for k in range(C)],
                      reads=[slab, x2], writes=[ps])
                v_ = vt.get()
                tr.op('act', lambda a, ps=ps, v_=v_: a.copy(out=v_[:, :], in_=ps[:, :]), reads=[ps], writes=[v_])
                tr.dma('pool', V3[t0 + b * 128:t0 + (b + 1) * 128, :], v_[:, :], reads=[v_], writes=[('V3', t0)])
        return run

    def tail_store_xb(es, wr=None):
        def run(x2, t0, si):
            pass
        return run

    def tail_final(es, wr=None):
        ot = sb(es, "t_fo", [128, D], F32, 2)

        def run(x2f, t0, si):
            for b in range(4):
                o_ = ot.get()
                for c0 in range(0, C, 4):
                    ps = PS.get()
                    tr.mm([(ps[:, j * 128:(j + 1) * 128], x2f[:, c0 + j, b * 128:(b + 1) * 128], IDF[:, :]) for j in range(4)],
                          reads=[x2f, IDF], writes=[ps], transpose=True)
                    if (c0 // 4) % 2 == 0:
                        tr.op('dve', lambda v, ps=ps, c0=c0, o_=o_: v.tensor_copy(out=o_[:, c0 * 128:(c0 + 4) * 128], in_=ps[:, :]),
                              reads=[ps], writes=[o_])
                    else:
                        tr.op('act', lambda a, ps=ps, c0=c0, o_=o_: a.copy(out=o_[:, c0 * 128:(c0 + 4) * 128], in_=ps[:, :]),
                              reads=[ps], writes=[o_])
                tr.dma('pool', Y[t0 + b * 128:t0 + (b + 1) * 128, :], o_[:, :], reads=[o_], writes=[('Y', t0, b)])
        return run

    def stage_ffn(L, tail_maker, final=False):
        with ExitStack() as es:
            A = sb(es, "f_a", [128, C, TT], BF16, 3)
            H = sb(es, "f_h", [128, 32, TT], BF16)
            S = sb(es, "f_s", [128, C, TT], F32)
            wr = sb(es, "f_w", [128, 8192], BF16, 2)
            tmp = sb(es, "f_tmp", [128, TT], F32, 4)
            lnb = ln_bufs(es)
            tail = tail_maker(es, wr)
            wg, wu, wd = WB[f'l{L}_ffn_w_gate'], WB[f'l{L}_ffn_w_up'], WB[f'l{L}_ffn_w_down']
            for (t0, si, s0, sl) in tiles:
                x1 = A.get()
                tr.dma('sp', x1[:, :, :], fm_view(XB1, t0, TT), reads=[('XB1', t0)], writes=[x1])
                groups = [[wg[0, n0 // 256], wu[0, n0 // 256]] for n0 in range(0, 4096, 256)]
                hold = {}

                def epi(gi, j, ps):
                    if j < 2:
                        hold[j] = ps
                        return
                    hc = 2 * gi + (j - 2)
                    pg = hold.pop(j - 2)
                    s_ = tmp.get()
                    tr.op('act', lambda a: a.activation(out=s_[:, :], in_=pg[:, :], func=AF.Silu), reads=[pg], writes=[s_])
                    tr.op('dve', lambda v: v.tensor_tensor(out=H[:, hc, :], in0=s_[:, :], in1=ps[:, :], op=ALU.mult),
                          reads=[s_, ps], writes=[H])
                linear(wr, C, groups, lambda k: x1[:, k, :], [x1], epi)
                groups = [[wd[0, n0 // 256]] for n0 in range(0, D, 256)]

                def epi2(gi, j, ps):
                    c = gi * 2 + j
                    tr.op('dve', lambda v: v.scalar_tensor_tensor(out=S[:, c, :], in0=x1[:, c, :], scalar=ALPHA, in1=ps[:, :],
                                                                  op0=ALU.mult, op1=ALU.add), reads=[x1, ps], writes=[S])
                linear(wr, 32, groups, lambda k: H[:, k, :], [H], epi2)
                x2 = A.get()
                layernorm(lnb, S, f'l{L}_ln2_g', f'l{L}_ln2_b', x2)
                tr.dma('pool', fm_view(XB, t0, TT), x2[:, :, :], reads=[x2], writes=[('XB', t0)])
                tail(x2, t0, si)
            tr.barrier()

    def stage_moe(L, tail_maker, final=False):
        with ExitStack() as es:
            A = sb(es, "m_a", [128, C, TT], BF16, 1 if final else 2)
            H = sb(es, "m_h", [128, 32, TT], BF16)
            S = sb(es, "m_s", [128, C, TT], F32)
            wr = sb(es, "m_w", [128, 8192], BF16, 2)
            tmp = sb(es, "m_tmp", [128, TT], F32, 4)
            tmp2 = sb(es, "m_tmp2", [128, TT], F32, 4)
            GB = sb(es, "m_gb", [128, 8, TT], BF16)
            LG = sb(es, "m_lg", [128, 4, 8], F32)
            MX = sb(es, "m_mx", [128, 4, 8], F32)
            GT = sb(es, "m_gt", [128, 4, 8], F32)
            E1 = sb(es, "m_e1", [128, 4, 8], F32)
            W2 = sb(es, "m_w2", [128, 4, 2], F32)
            GTS = sb(es, "m_gts", [8, TT], F32)
            lnb = ln_bufs(es)
            tail = tail_maker(es, wr)
            wg, wu, wd = WB[f'l{L}_moe_w_gate'], WB[f'l{L}_moe_w_up'], WB[f'l{L}_moe_w_down']
            wrt = WR[L]
            for (t0, si, s0, sl) in tiles:
                x1 = A.get()
                tr.dma('sp', x1[:, :, :], fm_view(XB1, t0, TT), reads=[('XB1', t0)], writes=[x1])
                ps = PS.get()
                for b in range(4):
                    tr.mm([(ps[:, b * 8:(b + 1) * 8], x1[:, k, b * 128:(b + 1) * 128], wrt[:, k, :], k == 0, k == C - 1) for k in range(C)],
                          reads=[x1, wrt], writes=[ps])
                tr.op('act', lambda a, ps=ps: a.copy(out=LG[:, :, :], in_=ps[:, 0:32].rearrange("p (b e) -> p b e", e=8)),
                      reads=[ps], writes=[LG])
                for b in range(4):
                    tr.op('dve', lambda v, b=b: v.max(out=MX[:, b, :], in_=LG[:, b, :]), reads=[LG], writes=[MX])
                tr.op('dve', lambda v: v.tensor_tensor(out=W2[:, :, 0:1], in0=MX[:, :, 1:2], in1=MX[:, :, 0:1], op=ALU.subtract),
                      reads=[MX], writes=[W2])
                tr.op('act', lambda a: a.activation(out=W2[:, :, 1:2], in_=W2[:, :, 0:1], func=AF.Sigmoid), reads=[W2], writes=[W2])
                tr.op('dve', lambda v: v.tensor_scalar(out=W2[:, :, 0:1], in0=W2[:, :, 1:2], scalar1=-1.0, scalar2=1.0, op0=ALU.mult, op1=ALU.add),
                      reads=[W2], writes=[W2])
                for b in range(4):
                    tr.op('dve', lambda v, b=b: v.tensor_scalar(out=GT[:, b, :], in0=LG[:, b, :], scalar1=MX[:, b, 0:1], scalar2=W2[:, b, 0:1],
                                                                op0=ALU.is_equal, op1=ALU.mult), reads=[LG, MX, W2], writes=[GT])
                    tr.op('dve', lambda v, b=b: v.tensor_scalar(out=E1[:, b, :], in0=LG[:, b, :], scalar1=MX[:, b, 1:2], scalar2=W2[:, b, 1:2],
                                                                op0=ALU.is_equal, op1=ALU.mult), reads=[LG, MX, W2], writes=[E1])
                tr.op('dve', lambda v: v.tensor_tensor(out=GT[:, :, :], in0=GT[:, :, :], in1=E1[:, :, :], op=ALU.add),
                      reads=[GT, E1], writes=[GT])
                ps = PS.get()
                tr.mm([(ps[0:8, b * 128:(b + 1) * 128], GT[:, b, :], IDF[:, :]) for b in range(4)], reads=[GT, IDF], writes=[ps], transpose=True)
                tr.op('act', lambda a, ps=ps: a.copy(out=GTS[:, :], in_=ps[0:8, :]), reads=[ps], writes=[GTS])
                for e in range(8):
                    ps = PS.get()
                    tr.mm([(ps[:, :], SEL[:, e, :], GTS[:, :], True, True)], reads=[SEL, GTS], writes=[ps])
                    tr.op('act', lambda a, ps=ps, e=e: a.copy(out=GB[:, e, :], in_=ps[:, :]), reads=[ps], writes=[GB])
                for eg in range(2):
                    for el in range(4):
                        e = eg * 4 + el
                        groups = [[wg[e, n0 // 256], wu[e, n0 // 256]] for n0 in range(0, 1024, 256)]
                        hold = {}

                        def epi(gi, j, ps, e=e, el=el, hold=hold):
                            if j < 2:
                                hold[j] = ps
                                return
                            hc = el * 8 + 2 * gi + (j - 2)
                            pg = hold.pop(j - 2)
                            s_ = tmp.get()
                            tr.op('act', lambda a: a.activation(out=s_[:, :], in_=pg[:, :], func=AF.Silu), reads=[pg], writes=[s_])
                            u_ = tmp2.get()
                            tr.op('dve', lambda v: v.tensor_tensor(out=u_[:, :], in0=ps[:, :], in1=GB[:, e, :], op=ALU.mult),
                                  reads=[ps, GB], writes=[u_])
                            tr.op('pool', lambda g: g.tensor_tensor(out=H[:, hc, :], in0=s_[:, :], in1=u_[:, :], op=ALU.mult),
                                  reads=[s_, u_], writes=[H])
                        linear(wr, C, groups, lambda k: x1[:, k, :], [x1], epi)
                    groups = [[wd[eg, n0 // 256]] for n0 in range(0, D, 256)]

                    def epi2(gi, j, ps, eg=eg):
                        c = gi * 2 + j
                        if eg == 0:
                            tr.op('dve', lambda v: v.scalar_tensor_tensor(out=S[:, c, :], in0=x1[:, c, :], scalar=ALPHA, in1=ps[:, :],
                                                                          op0=ALU.mult, op1=ALU.add), reads=[x1, ps], writes=[S])
                        else:
                            tr.op('dve', lambda v: v.tensor_tensor(out=S[:, c, :], in0=S[:, c, :], in1=ps[:, :], op=ALU.add),
                                  reads=[S, ps], writes=[S])
                    linear(wr, 32, groups, lambda k: H[:, k, :], [H], epi2)
                if final:
                    layernorm(lnb, S, f'l{L}_ln2_g', f'l{L}_ln2_b', S)
                    tail(S, t0, si)
                else:
                    x2 = A.get()
                    layernorm(lnb, S, f'l{L}_ln2_g', f'l{L}_ln2_b', x2)
                    tr.dma('pool', fm_view(XB, t0, TT), x2[:, :, :], reads=[x2], writes=[('XB', t0)])
                    tail(x2, t0, si)
            tr.barrier()

    def stage_attn_out(L, W=TT):
        with ExitStack() as es:
            A = sb(es, "o_a", [128, C, TT], BF16, 4)
            S = sb(es, "o_s", [128, C, TT], F32, 2)
            wr = sb(es, "o_w", [128, 8192], BF16, 2)
            lnb = ln_bufs(es)
            wb = WB[f'l{L}_attn_w_out']
            for (t0, si, s0, sl) in tiles:
                o = A.get()
                tr.dma('sp', o[:, :, :], fm_view(OT, t0, TT), reads=[('OT', t0)], writes=[o])
                xres = A.get()
                tr.dma('sp', xres[:, :, :], fm_view(XB, t0, TT), reads=[('XB', t0)], writes=[xres])
                s = S.get()
                groups = [[wb[0, n0 // 512]] for n0 in range(0, D, 512)]

                def epi(gi, j, ps):
                    c = gi * 4 + j
                    tr.op('dve', lambda v: v.scalar_tensor_tensor(out=s[:, c, :], in0=xres[:, c, :], scalar=ALPHA, in1=ps[:, :],
                                                                  op0=ALU.mult, op1=ALU.add), reads=[xres, ps], writes=[s])
                linear(wr, C, groups, lambda k: o[:, k, :], [o], epi)
                x1 = A.get()
                layernorm(lnb, s, f'l{L}_ln1_g', f'l{L}_ln1_b', x1)
                tr.dma('pool', fm_view(XB1, t0, TT), x1[:, :, :], reads=[x1], writes=[('XB1', t0)])
            tr.barrier()

    def stage_diff_attn():
        OFF = SMAX - 128
        LS = 2 * SMAX - 128
        scale = 128.0 ** -0.5
        with ExitStack() as es:
            ES = sb(es, "d_es", [128, LS], BF16)
            ti = sb(es, "d_ti", [128, 2048], I32)
            tf = sb(es, "d_tf", [128, 2048], F32)
            KT2 = sb(es, "d_kt", [128, 2, SMAX], BF16)
            QT2 = sb(es, "d_qt", [128, 2, SMAX], BF16)
            VH = sb(es, "d_vh", [128, SMAX // 128, 256], BF16)
            EB = sb(es, "d_e", [128, 2, SMAX], BF16, 2)
            AB = sb(es, "d_a", [128, SMAX], BF16, 2)
            ATB = sb(es, "d_at", [128, SMAX], BF16, 2)
            OTT = sb(es, "d_ot", [128, 2, SMAX], BF16)
            sm = sb(es, "d_sm", [128, 8], F32, 4)
            osb = sb(es, "d_osb", [128, 256], F32, 2)
            junk = sb(es, "d_junk", [128, 256], F32, 2)
            onb = sb(es, "d_onb", [128, 256], BF16, 2)
            negM = sb(es, "d_negm", [128, 2], F32, 2)
            for h in range(8):
                slope = 2.0 ** (-(h + 1))
                for u0 in range(0, LS, 2048):
                    w = min(2048, LS - u0)
                    tr.op('pool', lambda g, u0=u0, w=w: g.iota(ti[:, 0:w], pattern=[[-1, w]], base=OFF - u0, channel_multiplier=1), writes=[ti])
                    tr.op('dve', lambda v, w=w: v.tensor_copy(out=tf[:, 0:w], in_=ti[:, 0:w]), reads=[ti], writes=[tf])
                    tr.op('dve', lambda v, w=w: v.scalar_tensor_tensor(out=tf[:, 0:w], in0=tf[:, 0:w], scalar=-1.0, in1=tf[:, 0:w],
                                                                       op0=ALU.mult, op1=ALU.max), reads=[tf], writes=[tf])
                    tr.op('act', lambda a, u0=u0, w=w, slope=slope: a.activation(out=ES[:, u0:u0 + w], in_=tf[:, 0:w], func=AF.Exp, scale=-slope),
                          reads=[tf], writes=[ES])
                for si in range(nseq):
                    s0, Sl = seq_starts[si], seq_lens[si]
                    nkb = Sl // 128
                    rk = [('QKT', t) for t in range(s0, s0 + Sl, TT)]
                    tr.dma('sp', QT2[:, :, 0:Sl], fm_view(QKT, s0, Sl, rows0=(2 * h) * 128, nch=2), reads=rk, writes=[QT2])
                    tr.dma('sp', KT2[:, :, 0:Sl], fm_view(QKT, s0, Sl, rows0=(16 + 2 * h) * 128, nch=2), reads=rk, writes=[KT2])
                    tr.dma('sp', VH[:, 0:nkb, :], VV[s0:s0 + Sl, h * 256:(h + 1) * 256].rearrange("(kb p) e -> p kb e", p=128),
                           reads=[('VV', t) for t in range(s0, s0 + Sl, TT)], writes=[VH])
                    nm = negM.get()
                    tr.op('dve', lambda v, nm=nm, si=si, h=h: v.tensor_tensor(out=nm[:, :], in0=NRM[:, si, 2 * h:2 * h + 2],
                                                                             in1=NRM[:, si, 16 + 2 * h:16 + 2 * h + 2], op=ALU.mult),
                          reads=[NRM], writes=[nm])
                    tr.op('act', lambda a, nm=nm: a.activation(out=nm[:, :], in_=nm[:, :], func=AF.Sqrt), reads=[nm], writes=[nm])
                    tr.op('dve', lambda v, nm=nm: v.tensor_scalar(out=nm[:, :], in0=nm[:, :], scalar1=-scale, scalar2=None, op0=ALU.mult),
                          reads=[nm], writes=[nm])
                    def phaseA(qb):
                            e_ = EB.get()
                            s_ = sm.get()
                            for c in range(2):
                                for kt in range(Sl // 512):
                                    ps = PS.get()
                                    tr.mm([(ps[:, :], QT2[:, c, qb * 128:(qb + 1) * 128], KT2[:, c, kt * 512:(kt + 1) * 512], True, True)],
                                          reads=[QT2, KT2], writes=[ps])
                                    tr.op('act', lambda a, ps=ps, c=c, kt=kt, e_=e_, nm=nm: a.activation(
                                        out=e_[:, c, kt * 512:(kt + 1) * 512], in_=ps[:, :], func=AF.Exp, bias=nm[:, c:c + 1], scale=scale),
                                        reads=[ps, nm], writes=[(e_, c)])
                                eo = OFF - qb * 128
                                tr.op('dve', lambda v, c=c, e_=e_, s_=s_, eo=eo, Sl=Sl: v.scalar_tensor_tensor(
                                    out=e_[:, c, 0:Sl], in0=e_[:, c, 0:Sl], scalar=1.0, in1=ES[:, eo:eo + Sl], op0=ALU.mult, op1=ALU.mult,
                                    accum_out=s_[:, c:c + 1]), reads=[(e_, c), ES], writes=[(e_, c), (s_, c)])
                            tr.op('dve', lambda v, s_=s_: v.reciprocal(out=s_[:, 2:4], in_=s_[:, 0:2]), reads=[(s_, 0), (s_, 1)], writes=[(s_, 2)])
                            tr.op('dve', lambda v, s_=s_: v.tensor_tensor(out=s_[:, 4:5], in0=s_[:, 0:1], in1=s_[:, 3:4], op=ALU.mult),
                                  reads=[(s_, 2), (s_, 0)], writes=[(s_, 4)])
                            tr.op('dve', lambda v, s_=s_: v.tensor_scalar(out=s_[:, 5:6], in0=s_[:, 4:5], scalar1=LAMC[:, 0:1], scalar2=-1.0,
                                                                          op0=ALU.mult, op1=ALU.mult), reads=[(s_, 4), LAMC], writes=[(s_, 5)])
                            a_ = AB.get()
                            tr.op('dve', lambda v, e_=e_, a_=a_, s_=s_, Sl=Sl: v.scalar_tensor_tensor(
                                out=a_[:, 0:Sl], in0=e_[:, 1, 0:Sl], scalar=s_[:, 5:6], in1=e_[:, 0, 0:Sl], op0=ALU.mult, op1=ALU.add),
                                reads=[(e_, 0), (e_, 1), (s_, 5)], writes=[a_])
                            return (s_, a_)

                    def phaseB(qb, st):
                            s_, a_ = st
                            at = ATB.get()
                            for k0 in range(0, nkb, 8):
                                nk = min(8, nkb - k0)
                                ps = PS.get()
                                psb = ps[:, :].bitcast(BF16)
                                tr.mm([(psb[:, j * 128:(j + 1) * 128], a_[:, (k0 + j) * 128:(k0 + j + 1) * 128], IDB[:, :]) for j in range(nk)],
                                      reads=[a_, IDB], writes=[ps], transpose=True)
                                tr.op('act', lambda a, psb=psb, at=at, k0=k0, nk=nk: a.copy(out=at[:, k0 * 128:(k0 + nk) * 128], in_=psb[:, 0:nk * 128]),
                                      reads=[ps], writes=[(at, k0)])
                            po = PS.get()
                            tr.mm([(po[:, 0:256], at[:, kb * 128:(kb + 1) * 128], VH[:, kb, :], kb == 0, kb == nkb - 1) for kb in range(nkb)],
                                  reads=[(at, k0) for k0 in range(0, nkb, 8)] + [VH], writes=[po])
                            o_ = osb.get()
                            tr.op('act', lambda a, po=po, o_=o_, s_=s_: a.activation(out=o_[:, :], in_=po[:, 0:256], func=AF.Identity, scale=s_[:, 2:3]),
                                  reads=[po, (s_, 2)], writes=[o_])
                            j_ = junk.get()
                            tr.op('act', lambda a, o_=o_, j_=j_, s_=s_: a.activation(out=j_[:, :], in_=o_[:, :], func=AF.Square, accum_out=s_[:, 6:7]),
                                  reads=[o_], writes=[j_, (s_, 6)])
                            tr.op('dve', lambda v, s_=s_: v.tensor_scalar(out=s_[:, 6:7], in0=s_[:, 6:7], scalar1=1.0 / 256.0, scalar2=1e-5,
                                                                          op0=ALU.mult, op1=ALU.add), reads=[(s_, 6)], writes=[(s_, 6)])
                            tr.op('act', lambda a, s_=s_: a.activation(out=s_[:, 7:8], in_=s_[:, 6:7], func=AF.Sqrt), reads=[(s_, 6)], writes=[(s_, 7)])
                            tr.op('dve', lambda v, s_=s_: v.reciprocal(out=s_[:, 7:8], in_=s_[:, 7:8]), reads=[(s_, 7)], writes=[(s_, 7)])
                            on = onb.get()
                            tr.op('dve', lambda v, o_=o_, on=on, s_=s_: v.scalar_tensor_tensor(out=on[:, :], in0=o_[:, :], scalar=s_[:, 7:8], in1=GROW[:, :],
                                                                                             op0=ALU.mult, op1=ALU.mult),
                                  reads=[o_, (s_, 7), GROW], writes=[on])
                            ps = PS.get()
                            psb = ps[:, :].bitcast(BF16)
                            tr.mm([(psb[:, j * 128:(j + 1) * 128], on[:, j * 128:(j + 1) * 128], IDB[:, :]) for j in range(2)],
                                  reads=[on, IDB], writes=[ps], transpose=True)
                            tr.op('act', lambda a, psb=psb, qb=qb: a.copy(out=OTT[:, :, qb * 128:(qb + 1) * 128],
                                                                           in_=psb[:, 0:256].rearrange("p (j q) -> p j q", j=2)),
                                  reads=[ps], writes=[OTT])

                    st_ = phaseA(0)
                    for qb in range(nkb):
                        nx_ = phaseA(qb + 1) if qb + 1 < nkb else None
                        phaseB(qb, st_)
                        st_ = nx_
                    tr.dma('pool', fm_view(OT, s0, Sl, rows0=2 * h * 128, nch=2), OTT[:, :, 0:Sl], reads=[OTT],
                           writes=[('OT', t) for t in range(s0, s0 + Sl, TT)])
            tr.barrier()

    def stage_win_attn():
        scale = 128.0 ** -0.5
        with ExitStack() as es:
            EW = sb(es, "w_ew", [128, 16, 384], BF16)
            ti = sb(es, "w_ti", [128, 384], I32)
            tf = sb(es, "w_tf", [128, 384], F32)
            tm = sb(es, "w_tm", [128, 384], F32)
            te = sb(es, "w_te", [128, 384], F32)
            QT = sb(es, "w_q", [128, 16, TT], BF16, 2)
            KW = sb(es, "w_k", [128, 4, TT + 256], BF16, 2)
            VW = sb(es, "w_v", [128, 6, 512], BF16, 2)
            EBf = sb(es, "w_e", [128, 384], BF16, 3)
            PT = sb(es, "w_pt", [128, 384], BF16, 3)
            OB = sb(es, "w_ob", [128, D], BF16, 2)
            OTt = sb(es, "w_ot", [128, C, TT], BF16, 2)
            sm = sb(es, "w_sm", [128, 4], F32, 6)
            NM = sb(es, "w_nm", [128, nseq, 16], F32)
            SK = sb(es, "w_sk", [128, nseq, 16], F32)
            tr.op('pool', lambda g: g.iota(ti[:, :], pattern=[[-1, 384]], base=128, channel_multiplier=1), writes=[ti])
            tr.op('dve', lambda v: v.tensor_copy(out=tf[:, :], in_=ti[:, :]), reads=[ti], writes=[tf])
            tr.op('dve', lambda v: v.scalar_tensor_tensor(out=tf[:, :], in0=tf[:, :], scalar=-1.0, in1=tf[:, :], op0=ALU.mult, op1=ALU.max),
                  reads=[tf], writes=[tf])
            tr.op('dve', lambda v: v.tensor_single_scalar(out=tm[:, :], in_=tf[:, :], scalar=128.0, op=ALU.is_le), reads=[tf], writes=[tm])
            for h in range(16):
                slope = 2.0 ** (-8.0 * (h + 1) / 16.0)
                tr.op('act', lambda a, slope=slope: a.activation(out=te[:, :], in_=tf[:, :], func=AF.Exp, scale=-slope), reads=[tf], writes=[te])
                tr.op('dve', lambda v, h=h: v.tensor_tensor(out=EW[:, h, :], in0=te[:, :], in1=tm[:, :], op=ALU.mult), reads=[te, tm], writes=[EW])
            for si in range(nseq):
                for kv in range(4):
                    tr.op('dve', lambda v, si=si, kv=kv: v.tensor_scalar(out=NM[:, si, kv * 4:(kv + 1) * 4], in0=NRM3[:, si, kv * 4:(kv + 1) * 4],
                                                                          scalar1=NRM3[:, si, 16 + kv:17 + kv], scalar2=None, op0=ALU.mult),
                          reads=[NRM3], writes=[NM])
            tr.op('act', lambda a: a.activation(out=NM[:, :, :], in_=NM[:, :, :], func=AF.Sqrt), reads=[NM], writes=[NM])
            tr.op('dve', lambda v: v.tensor_scalar(out=NM[:, :, :], in0=NM[:, :, :], scalar1=-scale, scalar2=None, op0=ALU.mult),
                  reads=[NM], writes=[NM])
            for si in range(nseq):
                tr.op('dve', lambda v, si=si: v.tensor_tensor(out=SK[:, si, :], in0=NM[:, si, :], in1=SINK[:, :], op=ALU.add),
                      reads=[NM, SINK], writes=[SK])
            tr.op('act', lambda a: a.activation(out=SK[:, :, :], in_=SK[:, :, :], func=AF.Exp), reads=[SK], writes=[SK])
            for (t0, si, s0, sl) in tiles:
                q = QT.get()
                tr.dma('sp', q[:, :, :], fm_view(Q3, t0, TT), reads=[('Q3', t0)], writes=[q])
                lo = max(t0 - 128, s0)
                hi = min(t0 + TT + 128, s0 + sl)
                off = lo - (t0 - 128)
                kw = KW.get()
                rk = [t for t in range((lo // TT) * TT, hi, TT)]
                tr.dma('sp', kw[:, :, off:off + hi - lo], fm_view(K3, lo, hi - lo, nch=4), reads=[('K3', t) for t in rk], writes=[kw])
                vw = VW.get()
                tr.dma('sp', vw[:, off // 128:(off + hi - lo) // 128, :], V3[lo:hi, :].rearrange("(b p) e -> p b e", p=128),
                       reads=[('V3', t) for t in rk], writes=[vw])
                ott = OTt.get()
                for b in range(4):
                    g0 = t0 + b * 128
                    c_lo = b * 128 + (128 if g0 - 128 < s0 else 0)
                    c_hi = b * 128 + 384 - (128 if g0 + 256 > s0 + sl else 0)
                    Wd = c_hi - c_lo
                    eo = c_lo - b * 128
                    ob = OB.get()
                    for h in range(16):
                        kv = h // 4
                        ps = PS.get()
                        tr.mm([(ps[:, 0:Wd], q[:, h, b * 128:(b + 1) * 128], kw[:, kv, c_lo:c_hi], True, True)], reads=[q, kw], writes=[ps])
                        e_ = EBf.get()
                        s_ = sm.get()
                        tr.op('act', lambda a, ps=ps, e_=e_, Wd=Wd, si=si, h=h: a.activation(out=e_[:, 0:Wd], in_=ps[:, 0:Wd], func=AF.Exp,
                                                                                           bias=NM[:, si, h:h + 1], scale=scale),
                              reads=[ps, NM], writes=[e_])
                        tr.op('dve', lambda v, e_=e_, s_=s_, Wd=Wd, eo=eo, h=h: v.scalar_tensor_tensor(
                            out=e_[:, 0:Wd], in0=e_[:, 0:Wd], scalar=1.0, in1=EW[:, h, eo:eo + Wd], op0=ALU.mult, op1=ALU.mult,
                            accum_out=s_[:, 0:1]), reads=[e_, EW], writes=[e_, s_])
                        tr.op('dve', lambda v, s_=s_, si=si, h=h: v.tensor_tensor(out=s_[:, 1:2], in0=s_[:, 0:1], in1=SK[:, si, h:h + 1], op=ALU.add),
                              reads=[s_, SK], writes=[s_])
                        tr.op('dve', lambda v, s_=s_: v.reciprocal(out=s_[:, 2:3], in_=s_[:, 1:2]), reads=[s_], writes=[s_])
                        nb_ = Wd // 128
                        pt = PT.get()
                        ps2 = PS.get()
                        psb = ps2[:, :].bitcast(BF16)
                        tr.mm([(psb[:, j * 128:(j + 1) * 128], e_[:, j * 128:(j + 1) * 128], IDB[:, :]) for j in range(nb_)],
                              reads=[e_, IDB], writes=[ps2], transpose=True)
                        tr.op('act', lambda a, psb=psb, pt=pt, Wd=Wd: a.copy(out=pt[:, 0:Wd], in_=psb[:, 0:Wd]), reads=[ps2], writes=[pt])
                        po = PS.get()
                        tr.mm([(po[:, 0:128], pt[:, j * 128:(j + 1) * 128], vw[:, c_lo // 128 + j, kv * 128:(kv + 1) * 128], j == 0, j == nb_ - 1)
                               for j in range(nb_)], reads=[pt, vw], writes=[po])
                        tr.op('act', lambda a, po=po, ob=ob, s_=s_, h=h: a.activation(out=ob[:, h * 128:(h + 1) * 128], in_=po[:, 0:128],
                                                                                      func=AF.Identity, scale=s_[:, 2:3]),
                              reads=[po, s_], writes=[ob])
                    for c0 in range(0, C, 8):
                        ps = PS.get()
                        psb = ps[:, :].bitcast(BF16)
                        tr.mm([(psb[:, j * 128:(j + 1) * 128], ob[:, (c0 + j) * 128:(c0 + j + 1) * 128], IDB[:, :]) for j in range(8)],
                              reads=[ob, IDB], writes=[ps], transpose=True)
                        tr.op('dve', lambda v, psb=psb, c0=c0, b=b: v.tensor_copy(out=ott[:, c0:c0 + 8, b * 128:(b + 1) * 128],
                                                                                  in_=psb[:, :].rearrange("p (j q) -> p j q", j=8)),
                              reads=[ps], writes=[ott])
                tr.dma('pool', fm_view(OT, t0, TT), ott[:, :, :], reads=[ott], writes=[('OT', t0)])
            tr.barrier()

    def stage_lru(dir_):
        TR = 256
        with ExitStack() as es:
            RT = sb(es, "r_rt", [128, C, TR + 3], F32, 1)
            RC = sb(es, "r_rc", [128, C, TR], F32, 1)
            RCB = sb(es, "r_rcb", [128, C, TR], BF16, 1)
            HS = sb(es, "r_hs", [128, C, TR], F32, 2)
            GS = sb(es, "r_gs", [128, 16, 256], BF16, 2)
            tg = sb(es, "r_tg", [128, TR], F32, 3)
            ta = sb(es, "r_ta", [128, TR], F32, 3)
            tb = sb(es, "r_tb", [128, TR], F32, 3)
            if dir_ == 1:
                wr = sb(es, "r_w", [128, 4096], BF16, 2)
                HFT = sb(es, "r_hf", [128, C, TR], F32, 1)
                YBT = sb(es, "r_yb", [128, C, TR], BF16, 1)
                MT = sb(es, "r_mt", [128, C, TR], BF16, 1)
                XR = sb(es, "r_xr", [128, C, TR], BF16, 1)
                S = sb(es, "r_s", [128, C, TR], F32, 1)
                X1 = sb(es, "r_x1", [128, C, TR], BF16, 1)
                tmp = sb(es, "r_tmp", [128, TR], F32, 2)
                lnb = ln_bufs(es, TR)
            cw = vidx['l2_rec_conv_w']
            cb = vidx['l2_rec_conv_b']
            gb = vidx['l2_rec_gate_b']
            bo = vidx['l2_rec_b_out']
            gw = WB['l2_rec_gate_w']
            for si in range(nseq):
                s0, Sl = seq_starts[si], seq_lens[si]
                nt = Sl // TR
                order = range(nt) if dir_ == 0 else range(nt - 1, -1, -1)
                prev = None
                for ti_ in order:
                    t0 = s0 + ti_ * TR
                    rt = RT.get()
                    lo = max(t0 - 2, s0)
                    hi = min(t0 + TR + 1, s0 + Sl)
                    if lo > t0 - 2:
                        tr.op('pool', lambda g, rt=rt: g.memset(rt[:, :, 0:2], 0.0), writes=[rt])
                    if hi < t0 + TR + 1:
                        tr.op('pool', lambda g, rt=rt: g.memset(rt[:, :, TR + 2:TR + 3], 0.0), writes=[rt])
                    off = lo - (t0 - 2)
                    tr.dma('sp', rt[:, :, off:off + hi - lo], fm_view(RB, lo, hi - lo),
                           reads=[('RB', t) for t in range((lo // TT) * TT, hi, TT)], writes=[rt])
                    rc = RC.get()
                    rcb = RCB.get()
                    for c in range(C):
                        tr.op('dve', lambda v, c=c: v.tensor_scalar(out=rc[:, c, :], in0=rt[:, c, 0:TR], scalar1=VEC[:, cw, c:c + 1],
                                                                    scalar2=VEC[:, cb, c:c + 1], op0=ALU.mult, op1=ALU.add),
                              reads=[rt, VEC], writes=[(rc, c)])
                    for k in range(1, 4):
                        for c in range(C):
                            tr.op('dve', lambda v, c=c, k=k: v.scalar_tensor_tensor(out=rc[:, c, :], in0=rt[:, c, k:k + TR], scalar=VEC[:, cw + k, c:c + 1],
                                                                                   in1=rc[:, c, :], op0=ALU.mult, op1=ALU.add),
                                  reads=[rt, VEC, (rc, c)], writes=[(rc, c)])
                    for c in range(C):
                        tr.op('pool', lambda g, c=c: g.tensor_copy(out=rcb[:, c, :], in_=rc[:, c, :]), reads=[(rc, c)], writes=[(rcb, c)])
                    hs = HS.get()
                    slabs = []
                    for g_ in range(2):
                        sl_ = GS.get()
                        base = (dir_ * 2 + g_) * 2048
                        tr.dma('sp', sl_[:, :, :], gw[dir_ * 2 + g_, 0].rearrange("p (j o) -> p j o", o=256), writes=[sl_])
                        slabs.append(sl_)
                    for oc in range(C):
                        n_, ol = oc // 2, oc % 2
                        pss = []
                        for g_ in range(2):
                            ps = PS.get()
                            tr.mm([(ps[:, 0:TR], slabs[g_][:, n_ * 2 + kc, ol * 128:(ol + 1) * 128], rcb[:, 2 * n_ + kc, :], kc == 0, kc == 1) for kc in range(2)],
                                  reads=[slabs[g_], (rcb, 2 * n_), (rcb, 2 * n_ + 1)], writes=[ps])
                            pss.append(ps)
                        r_ = tg.get()
                        tr.op('act', lambda a, r_=r_, ps=pss[0], oc=oc: a.activation(out=r_[:, :], in_=ps[:, 0:TR], func=AF.Exp, scale=-1.0,
                                                                                   bias=NGB[:, dir_ * 2, oc:oc + 1]), reads=[pss[0], NGB], writes=[r_])
                        tr.op('dve', lambda v, r_=r_: v.tensor_scalar(out=r_[:, :], in0=r_[:, :], scalar1=1.0, scalar2=None, op0=ALU.add), reads=[r_], writes=[r_])
                        tr.op('dve', lambda v, r_=r_: v.reciprocal(out=r_[:, :], in_=r_[:, :]), reads=[r_], writes=[r_])
                        a_ = ta.get()
                        tr.op('act', lambda a, r_=r_, a_=a_, oc=oc: a.activation(out=a_[:, :], in_=r_[:, :], func=AF.Exp, scale=NSP[:, dir_, oc:oc + 1]),
                              reads=[r_, NSP], writes=[a_])
                        i_ = tg.get()
                        tr.op('act', lambda a, i_=i_, ps=pss[1], oc=oc: a.activation(out=i_[:, :], in_=ps[:, 0:TR], func=AF.Exp, scale=-1.0,
                                                                                   bias=NGB[:, dir_ * 2 + 1, oc:oc + 1]), reads=[pss[1], NGB], writes=[i_])
                        tr.op('dve', lambda v, i_=i_: v.tensor_scalar(out=i_[:, :], in0=i_[:, :], scalar1=1.0, scalar2=None, op0=ALU.add), reads=[i_], writes=[i_])
                        tr.op('dve', lambda v, i_=i_: v.reciprocal(out=i_[:, :], in_=i_[:, :]), reads=[i_], writes=[i_])
                        q_ = tb.get()
                        tr.op('dve', lambda v, q_=q_, a_=a_: v.scalar_tensor_tensor(out=q_[:, :], in0=a_[:, :], scalar=-1.0, in1=a_[:, :], op0=ALU.mult, op1=ALU.mult),
                              reads=[a_], writes=[q_])
                        tr.op('dve', lambda v, q_=q_: v.tensor_scalar(out=q_[:, :], in0=q_[:, :], scalar1=1.0, scalar2=1e-30, op0=ALU.add, op1=ALU.max),
                              reads=[q_], writes=[q_])
                        tr.op('act', lambda a, q_=q_: a.activation(out=q_[:, :], in_=q_[:, :], func=AF.Sqrt), reads=[q_], writes=[q_])
                        tr.op('dve', lambda v, i_=i_, oc=oc: v.tensor_tensor(out=i_[:, :], in0=i_[:, :], in1=rc[:, oc, :], op=ALU.mult),
                              reads=[i_, (rc, oc)], writes=[i_])
                        tr.op('dve', lambda v, i_=i_, q_=q_: v.tensor_tensor(out=i_[:, :], in0=i_[:, :], in1=q_[:, :], op=ALU.mult),
                              reads=[i_, q_], writes=[i_])
                        if dir_ == 0:
                            init = 0.0 if prev is None else prev[:, oc, TR - 1:TR]
                            tr.op('dve', lambda v, a_=a_, i_=i_, oc=oc, init=init: v.tensor_tensor_scan(out=hs[:, oc, :], data0=a_[:, :], data1=i_[:, :],
                                                                                                   initial=init, op0=ALU.mult, op1=ALU.add),
                                  reads=[a_, i_] + ([] if prev is None else [(prev, oc)]), writes=[(hs, oc)])
                        else:
                            init = 0.0 if prev is None else prev[:, oc, 0:1]
                            tr.op('dve', lambda v, a_=a_, i_=i_, oc=oc, init=init: v.tensor_tensor_scan(out=hs[:, oc, ::-1], data0=a_[:, ::-1], data1=i_[:, ::-1],
                                                                                                   initial=init, op0=ALU.mult, op1=ALU.add),
                                  reads=[a_, i_] + ([] if prev is None else [(prev, oc)]), writes=[(hs, oc)])
                    hkeys = [(hs, c) for c in range(C)]
                    if dir_ == 0:
                        tr.dma('pool', fm_view(HF, t0, TR), hs[:, :, :], reads=hkeys, writes=[('HF', t0)])
                    else:
                        hf = HFT.get()
                        tr.dma('sp', hf[:, :, :], fm_view(HF, t0, TR), reads=[('HF', t0)], writes=[hf])
                        yb = YBT.get()
                        tr.dma('sp', yb[:, :, :], fm_view(YB, t0, TR), reads=[('YB', (t0 // TT) * TT)], writes=[yb])
                        xr = XR.get()
                        tr.dma('sp', xr[:, :, :], fm_view(XB, t0, TR), reads=[('XB', (t0 // TT) * TT)], writes=[xr])
                        mt = MT.get()
                        for c in range(C):
                            tr.op('pool', lambda g, c=c, hf=hf: g.tensor_tensor(out=hf[:, c, :], in0=hf[:, c, :], in1=hs[:, c, :], op=ALU.add),
                                  reads=[hf, (hs, c)], writes=[hf])
                            tr.op('dve', lambda v, c=c, hf=hf, yb=yb, mt=mt: v.tensor_tensor(out=mt[:, c, :], in0=hf[:, c, :], in1=yb[:, c, :], op=ALU.mult),
                                  reads=[hf, yb], writes=[mt])
                        s = S.get()
                        wb = WB['l2_rec_w_out']
                        groups = [[wb[0, n0 // 256]] for n0 in range(0, D, 256)]

                        def epi(gi, j, ps, xr=xr, s=s):
                            c = gi * 2 + j
                            t_ = tmp.get()
                            tr.op('act', lambda a: a.activation(out=t_[:, :], in_=ps[:, 0:TR], func=AF.Identity, bias=VEC[:, bo, c:c + 1]),
                                  reads=[ps, VEC], writes=[t_])
                            tr.op('dve', lambda v: v.scalar_tensor_tensor(out=s[:, c, :], in0=xr[:, c, :], scalar=ALPHA, in1=t_[:, :],
                                                                          op0=ALU.mult, op1=ALU.add), reads=[xr, t_], writes=[s])
                        linear(wr, C, groups, lambda k, mt=mt: mt[:, k, :], [mt], epi, W=TR)
                        x1 = X1.get()
                        layernorm(lnb, s, 'l2_ln1_g', 'l2_ln1_b', x1, W=TR)
                        tr.dma('pool', fm_view(XB1, t0, TR), x1[:, :, :], reads=[x1], writes=[('XB1', (t0 // TT) * TT, t0)])
                    prev = hs
            tr.barrier()

    def stage_dump(src):
        with ExitStack() as es:
            A = sb(es, "z_a", [128, C, TT], BF16, 2)
            AF_ = sb(es, "z_f", [128, C, TT], F32, 2)
            tail = tail_final(es)
            for (t0, si, s0, sl) in tiles:
                a = A.get()
                tr.dma('sp', a[:, :, :], fm_view(src, t0, TT), writes=[a])
                f = AF_.get()
                tr.op('dve', lambda v, a=a, f=f: v.tensor_copy(out=f[:, :, :], in_=a[:, :, :]), reads=[a], writes=[f])
                tail(f, t0, si)
            tr.barrier()

    setup()
    L0 = ['l0_conv_w_in', 'l0_conv_w_out', 'l0_ffn_w_gate', 'l0_ffn_w_up', 'l0_ffn_w_down', 'l1_attn_w_qkv']
    L1 = ['l1_attn_w_out', 'l1_moe_w_gate', 'l1_moe_w_up', 'l1_moe_w_down', 'l2_rec_w_in']
    L2 = ['l2_rec_gate_w', 'l2_rec_w_out', 'l2_ffn_w_gate', 'l2_ffn_w_up', 'l2_ffn_w_down', 'l3_attn_w_qkv']
    L3 = ['l3_attn_w_out', 'l3_moe_w_gate', 'l3_moe_w_up', 'l3_moe_w_down']
    convert(L0)
    stage_l0a()
    stage_l0b()
    def prog():
        if n_layers == 1:
            stage_ffn(0, tail_store_xb)
            return stage_dump(XB)
        stage_ffn(0, tail_l1_qkv)
        if n_layers == 1.25:
            return stage_dump(XB)
        convert(L1)
        stage_diff_attn()
        if n_layers == 1.5:
            return stage_dump(OT)
        stage_attn_out(1)
        if n_layers == 1.75:
            return stage_dump(XB1)
        if n_layers == 2:
            stage_moe(1, tail_store_xb)
            return stage_dump(XB)
        stage_moe(1, tail_l2_win)
        convert(L2)
        stage_lru(0)
        if n_layers == 2.5:
            return stage_dump(XB)
        stage_lru(1)
        if n_layers == 2.75:
            return stage_dump(XB1)
        if n_layers == 3:
            stage_ffn(2, tail_store_xb)
            return stage_dump(XB)
        stage_ffn(2, tail_l3_qkv)
        convert(L3)
        stage_win_attn()
        if n_layers == 3.5:
            return stage_dump(OT)
        stage_attn_out(3)
        if n_layers == 3.75:
            return stage_dump(XB1)
        stage_moe(3, tail_final, final=True)
    prog()
    tr.barrier(engines=['sp'])
    return nc, tr


_CACHE = {}


def run_cores(x_cores, weights, seq_lens, n_layers=4, n_cores=N_CORES, trace=False):
    key = (tuple(seq_lens), n_layers)
    nc, tr = build(list(seq_lens), n_layers)
    in_maps = []
    for ci in range(n_cores):
        m = {"x": np.ascontiguousarray(x_cores[ci])}
        for n in WSHAPES:
            m[n] = weights[n]
        in_maps.append(m)
    res = run_bass_kernel_spmd(nc, in_maps, core_ids=list(range(n_cores)), trace=trace)
    return [r["y"] for r in res.results], res


def kernel(**inputs):
    xp = np.asarray(inputs['x_prompt'], dtype=np.float32)
    xs = np.asarray(inputs['x_sample'], dtype=np.float32)
    weights = {n: np.ascontiguousarray(np.asarray(inputs[n], dtype=np.float32)) for n in WSHAPES}
    x_cores = []
    for ci in range(N_CORES):
        x_cores.append(np.concatenate([xp[2 * ci].reshape(-1, D), xp[2 * ci + 1].reshape(-1, D), xs[ci].reshape(-1, D)], axis=0))
    ys, _ = run_cores(x_cores, weights, [2048, 2048, 4096])
    yp = np.empty_like(xp)
    ysamp = np.empty_like(xs)
    for ci in range(N_CORES):
        y = ys[ci]
        yp[2 * ci] = y[0:2048]
        yp[2 * ci + 1] = y[2048:4096]
        ysamp[ci] = y[4096:8192]
    return (yp, ysamp)
```

```python
import math
from contextlib import ExitStack
import numpy as np
import concourse.bass as bass
import concourse.mybir as mybir
from concourse.bass_utils import run_bass_kernel_spmd

F32 = mybir.dt.float32
BF16 = mybir.dt.bfloat16
I32 = mybir.dt.int32
AF = mybir.ActivationFunctionType
ALU = mybir.AluOpType
AX = mybir.AxisListType

D = 2048
C = 16
TT = 512
ALPHA = 8.0 ** 0.25
LN_EPS = 1e-5
N_CORES = 8
import os
DBG = os.environ.get('KDBG', 'qnv2')

WSHAPES = {
    'l0_conv_w_in': (2048, 4096), 'l0_conv_b_in': (4096,), 'l0_conv_w_dw': (31, 2048), 'l0_conv_b_dw': (2048,),
    'l0_conv_norm_g': (2048,), 'l0_conv_norm_b': (2048,), 'l0_conv_w_out': (2048, 2048), 'l0_conv_b_out': (2048,),
    'l0_ln1_g': (2048,), 'l0_ln1_b': (2048,), 'l0_ffn_w_gate': (2048, 4096), 'l0_ffn_w_up': (2048, 4096),
    'l0_ffn_w_down': (4096, 2048), 'l0_ln2_g': (2048,), 'l0_ln2_b': (2048,),
    'l1_attn_w_qkv': (2048, 6144), 'l1_attn_lambda': (4, 128), 'l1_attn_subln_g': (256,), 'l1_attn_w_out': (2048, 2048),
    'l1_ln1_g': (2048,), 'l1_ln1_b': (2048,), 'l1_moe_w_router': (2048, 8), 'l1_moe_w_gate': (8, 2048, 1024),
    'l1_moe_w_up': (8, 2048, 1024), 'l1_moe_w_down': (8, 1024, 2048), 'l1_ln2_g': (2048,), 'l1_ln2_b': (2048,),
    'l2_rec_w_in': (2048, 4096), 'l2_rec_b_in': (4096,), 'l2_rec_conv_w': (4, 2048), 'l2_rec_conv_b': (2048,),
    'l2_rec_gate_w': (2, 2, 8, 256, 256), 'l2_rec_gate_b': (2, 2, 2048), 'l2_rec_lambda': (2, 2048),
    'l2_rec_w_out': (2048, 2048), 'l2_rec_b_out': (2048,), 'l2_ln1_g': (2048,), 'l2_ln1_b': (2048,),
    'l2_ffn_w_gate': (2048, 4096), 'l2_ffn_w_up': (2048, 4096), 'l2_ffn_w_down': (4096, 2048),
    'l2_ln2_g': (2048,), 'l2_ln2_b': (2048,),
    'l3_attn_w_qkv': (2048, 3072), 'l3_attn_sink': (16,), 'l3_attn_w_out': (2048, 2048),
    'l3_ln1_g': (2048,), 'l3_ln1_b': (2048,), 'l3_moe_w_router': (2048, 8), 'l3_moe_w_gate': (8, 2048, 1024),
    'l3_moe_w_up': (8, 2048, 1024), 'l3_moe_w_down': (8, 1024, 2048), 'l3_ln2_g': (2048,), 'l3_ln2_b': (2048,),
}
MATS = {
    'l0_conv_w_in': (2048, 4096, None, 2048, 256), 'l0_conv_w_out': (2048, 2048, None, 2048, 512),
    'l0_ffn_w_gate': (2048, 4096, None, 2048, 256), 'l0_ffn_w_up': (2048, 4096, None, 2048, 256),
    'l0_ffn_w_down': (4096, 2048, None, 4096, 256),
    'l1_attn_w_qkv': (2048, 6144, None, 2048, 512), 'l1_attn_w_out': (2048, 2048, None, 2048, 512),
    'l1_moe_w_gate': (16384, 1024, "e k n -> (e k) n", 2048, 256), 'l1_moe_w_up': (16384, 1024, "e k n -> (e k) n", 2048, 256),
    'l1_moe_w_down': (8192, 2048, "e k n -> (e k) n", 4096, 256),
    'l2_rec_w_in': (2048, 4096, None, 2048, 512), 'l2_rec_gate_w': (8192, 256, "a b n i o -> (a b n i) o", 2048, 256),
    'l2_rec_w_out': (2048, 2048, None, 2048, 256), 'l2_ffn_w_gate': (2048, 4096, None, 2048, 256),
    'l2_ffn_w_up': (2048, 4096, None, 2048, 256), 'l2_ffn_w_down': (4096, 2048, None, 4096, 256),
    'l3_attn_w_qkv': (2048, 3072, None, 2048, 512), 'l3_attn_w_out': (2048, 2048, None, 2048, 512),
    'l3_moe_w_gate': (16384, 1024, "e k n -> (e k) n", 2048, 256), 'l3_moe_w_up': (16384, 1024, "e k n -> (e k) n", 2048, 256),
    'l3_moe_w_down': (8192, 2048, "e k n -> (e k) n", 4096, 256),
}
VECS = ['l0_conv_b_in', 'l0_conv_w_dw', 'l0_conv_b_dw', 'l0_conv_norm_g', 'l0_conv_norm_b', 'l0_conv_b_out',
        'l0_ln1_g', 'l0_ln1_b', 'l0_ln2_g', 'l0_ln2_b', 'l1_ln1_g', 'l1_ln1_b', 'l1_ln2_g', 'l1_ln2_b',
        'l2_rec_b_in', 'l2_rec_conv_w', 'l2_rec_conv_b', 'l2_rec_gate_b', 'l2_rec_lambda', 'l2_rec_b_out',
        'l2_ln1_g', 'l2_ln1_b', 'l2_ln2_g', 'l2_ln2_b', 'l3_ln1_g', 'l3_ln1_b', 'l3_ln2_g', 'l3_ln2_b']


class Buf:
    __slots__ = ('t',)

    def __init__(self, t):
        self.t = t

    def __getitem__(self, idx):
        return self.t[idx]

    def get(self):
        return self


class Ring:
    def __init__(self, bufs):
        self.bufs = bufs
        self.i = 0

    def get(self):
        b = self.bufs[self.i % len(self.bufs)]
        self.i += 1
        return b


class Tr:
    ENG = ('pe', 'act', 'dve', 'pool', 'sp')

    def __init__(self, nc):
        self.nc = nc
        self.eng = dict(pe=nc.tensor, act=nc.scalar, dve=nc.vector, pool=nc.gpsimd, sp=nc.sync)
        self.esem = {e: (nc.alloc_semaphore("es_" + e), "es_" + e) for e in ('pe', 'act', 'dve', 'pool')}
        self.ecnt = {e: 0 for e in self.esem}
        self.waited = {e: {} for e in self.ENG}
        self.res = {}
        self.dsem = {'sp': [(nc.alloc_semaphore(f"dsp{i}"), f"dsp{i}") for i in range(20)],
                     'pool': [(nc.alloc_semaphore(f"dpl{i}"), f"dpl{i}") for i in range(12)]}
        self.dval = {}
        self.di = {'sp': 0, 'pool': 0}
        self.ninst = 0

    def _need(self, e, ev):
        sem, name, val, src = ev
        if src == 'pe' and e == 'pe':
            return
        if self.waited[e].get(name, 0) >= val:
            return
        self.eng[e].wait_ge(sem, val)
        self.waited[e][name] = val
        self.ninst += 1

    def _deps(self, e, reads, writes):
        for k in reads:
            r = self.res.get(k)
            if r is not None and r[0] is not None:
                self._need(e, r[0])
        for k in writes:
            r = self.res.get(k)
            if r is not None:
                if r[0] is not None:
                    self._need(e, r[0])
                for ev in r[1].values():
                    self._need(e, ev)

    def _record(self, ev, reads, writes):
        for k in reads:
            r = self.res.get(k)
            if r is None:
                r = self.res[k] = [None, {}]
            r[1][ev[1]] = ev
        for k in writes:
            self.res[k] = [ev, {}]

    def op(self, e, fn, reads=(), writes=()):
        self._deps(e, reads, writes)
        ins = fn(self.eng[e])
        self.ecnt[e] += 1
        sem, name = self.esem[e]
        ins.then_inc(sem, 1)
        self._record((sem, name, self.ecnt[e], e), reads, writes)
        self.ninst += 1

    def mm(self, items, reads, writes, transpose=False):
        self._deps('pe', reads, writes)
        pe = self.eng['pe']
        ins = None
        for it in items:
            if transpose:
                ins = pe.transpose(it[0], it[1], it[2])
            else:
                ins = pe.matmul(it[0], it[1], it[2], start=it[3], stop=it[4])
        self.ecnt['pe'] += 1
        sem, name = self.esem['pe']
        ins.then_inc(sem, 1)
        self._record((sem, name, self.ecnt['pe'], 'pe'), reads, writes)
        self.ninst += len(items)

    def dma(self, q, out, in_, reads=(), writes=()):
        sems = self.dsem[q]
        i = self.di[q]
        self.di[q] += 1
        sem, name = sems[i % len(sems)]
        prev = self.dval.get(name, 0)
        if prev:
            self._need(q, (sem, name, prev, 'dma'))
        self._deps(q, reads, writes)
        self.eng[q].dma_start(out=out, in_=in_).then_inc(sem, 16)
        self.dval[name] = prev + 16
        self._record((sem, name, prev + 16, 'dma'), reads, writes)
        self.ninst += 1

    def barrier(self, engines=None):
        evs = []
        for e, (sem, name) in self.esem.items():
            if self.ecnt[e]:
                evs.append((sem, name, self.ecnt[e], 'x'))
        for q in self.dsem:
            for sem, name in self.dsem[q]:
                v = self.dval.get(name, 0)
                if v:
                    evs.append((sem, name, v, 'dma'))
        for e in (engines or self.ENG):
            for ev in evs:
                self._need(e, ev)
        if engines is None:
            self.res = {}


def build(seq_lens, n_layers=4):
    NT = sum(seq_lens)
    SMAX = max(seq_lens)
    seq_starts = [sum(seq_lens[:i]) for i in range(len(seq_lens))]
    nseq = len(seq_lens)
    nc = bass.Bass("TRN2", target_bir_lowering=False)
    tr = Tr(nc)
    gstack = ExitStack()

    X = nc.dram_tensor("x", [NT, D], F32, kind="ExternalInput").ap()
    Y = nc.dram_tensor("y", [NT, D], F32, kind="ExternalOutput").ap()
    WIN = {n: nc.dram_tensor(n, list(s), F32, kind="ExternalInput").ap() for n, s in WSHAPES.items()}

    def scr(name, shape, dt):
        return nc.dram_tensor(name, list(shape), dt, kind="Internal").ap()

    WB = {n: scr(n + "_bf", [r // rg, c // sw, 128, (rg // 128) * sw], BF16) for n, (r, c, _, rg, sw) in MATS.items()}
    XB = scr("XB", [D, NT], BF16)
    XB1 = scr("XB1", [D, NT], BF16)
    GLU = scr("GLU", [D, NT], BF16)
    QKT = scr("QKT", [4096, NT], BF16)
    VV = scr("VV", [NT, 2048], BF16)
    OT = scr("OT", [D, NT], BF16)
    YB = scr("YB", [D, NT], BF16)
    RB = scr("RB", [D, NT], F32)
    HF = scr("HF", [D, NT], F32)
    Q3 = scr("Q3", [D, NT], BF16)
    K3 = scr("K3", [512, NT], BF16)
    V3 = scr("V3", [NT, 512], BF16)
    DIAG = scr("DIAG", [C, 128, 31 * 128], BF16)

    uid = [0]

    def sb(es, name, shape, dt, n=1):
        uid[0] += 1
        bufs = [Buf(es.enter_context(nc.sbuf_tensor(f"{name}_{uid[0]}_{i}", list(shape), dt))) for i in range(n)]
        return bufs[0] if n == 1 else Ring(bufs)

    PS = Ring([Buf(gstack.enter_context(nc.psum_tensor(f"ps{i}", [128, 512], F32))) for i in range(8)])
    nvec = sum(int(np.prod(WSHAPES[n])) // 2048 for n in VECS)
    VEC = sb(gstack, "vec", [128, nvec, 16], F32)
    vidx = {}
    _o = 0
    for n in VECS:
        vidx[n] = _o
        _o += int(np.prod(WSHAPES[n])) // 2048
    IDF = sb(gstack, "idf", [128, 128], F32)
    IDB = sb(gstack, "idb", [128, 128], BF16)
    ONESM = sb(gstack, "onesm", [128, 128], BF16)
    ONES1 = sb(gstack, "ones1", [128, 128], BF16)
    CHALF = sb(gstack, "chalf", [128, 512], F32)
    CNHALF = sb(gstack, "cnhalf", [128, 512], F32)
    NRM = sb(gstack, "nrm", [128, nseq, 32], F32)
    NRM3 = sb(gstack, "nrm3", [128, nseq, 20], F32)
    NSP = sb(gstack, "nsp", [128, 2, 16], F32)
    LAMC = sb(gstack, "lamc", [128, 1], F32)
    GROW = sb(gstack, "grow", [128, 256], F32)
    SINK = sb(gstack, "sink", [128, 16], F32)
    SEL = sb(gstack, "sel", [8, 8, 128], F32)
    WR = {1: sb(gstack, "wr1", [128, 16, 8], BF16), 3: sb(gstack, "wr3", [128, 16, 8], BF16)}

    def vcol(name, j, c):
        return VEC[:, vidx[name] + j, c:c + 1]

    def setup():
        with ExitStack() as es:
            ti = sb(es, "s_ti", [128, 512], I32)
            tf = sb(es, "s_tf", [128, 512], F32)
            tr.op('pool', lambda g: g.iota(ti[:, 0:128], pattern=[[-1, 128]], base=0, channel_multiplier=1), writes=[ti])
            tr.op('dve', lambda v: v.tensor_copy(out=tf[:, 0:128], in_=ti[:, 0:128]), reads=[ti], writes=[tf])
            tr.op('dve', lambda v: v.tensor_single_scalar(out=IDF[:, :], in_=tf[:, 0:128], scalar=0.0, op=ALU.is_equal),
                  reads=[tf], writes=[IDF])
            tr.op('dve', lambda v: v.tensor_copy(out=IDB[:, :], in_=IDF[:, :]), reads=[IDF], writes=[IDB])
            tr.op('pool', lambda g: g.memset(ONESM[:, :], 1.0 / 2048.0), writes=[ONESM])
            tr.op('pool', lambda g: g.memset(ONES1[:, :], 1.0), writes=[ONES1])
            tr.op('pool', lambda g: g.memset(CHALF[:, :], 0.5), writes=[CHALF])
            tr.op('pool', lambda g: g.memset(CNHALF[:, :], -0.5), writes=[CNHALF])
            tr.op('pool', lambda g: g.memset(NRM[:, :, :], 0.0), writes=[NRM])
            tr.op('pool', lambda g: g.memset(NRM3[:, :, :], 0.0), writes=[NRM3])
            ti2 = sb(es, "s_ti2", [8, 8, 128], I32)
            tf2 = sb(es, "s_tf2", [8, 8, 128], F32)
            tr.op('pool', lambda g: g.iota(ti2[:, :, :], pattern=[[-1, 8], [0, 128]], base=0, channel_multiplier=1), writes=[ti2])
            tr.op('dve', lambda v: v.tensor_copy(out=tf2[:, :, :], in_=ti2[:, :, :]), reads=[ti2], writes=[tf2])
            tr.op('dve', lambda v: v.tensor_single_scalar(out=SEL[:, :, :], in_=tf2[:, :, :], scalar=0.0, op=ALU.is_equal),
                  reads=[tf2], writes=[SEL])
            stg = sb(es, "s_stg", [16, nvec, 128], F32)
            for n in VECS:
                k = int(np.prod(WSHAPES[n])) // 2048
                src = WIN[n]
                nd = len(WSHAPES[n])
                if nd == 1:
                    v2 = src.rearrange("(j c p) -> c j p", c=16, p=128)
                elif nd == 2:
                    v2 = src.rearrange("j (c p) -> c j p", p=128)
                else:
                    v2 = src.rearrange("a b (c p) -> c (a b) p", p=128)
                tr.dma('sp', stg[:, vidx[n]:vidx[n] + k, :], v2, writes=[stg])
            for v0 in range(0, nvec, 32):
                nv = min(32, nvec - v0)
                ps = PS.get()
                tr.mm([(ps[:, j * 16:(j + 1) * 16], stg[:, v0 + j, :], IDF[0:16, 0:16]) for j in range(nv)],
                      reads=[stg, IDF], writes=[ps], transpose=True)
                tr.op('dve', lambda v, ps=ps, v0=v0, nv=nv: v.tensor_copy(
                    out=VEC[:, v0:v0 + nv, :], in_=ps[:, 0:nv * 16].rearrange("p (j c) -> p j c", c=16)),
                    reads=[ps], writes=[VEC])
            for L in (1, 3):
                rt = sb(es, f"s_rt{L}", [128, 16, 8], F32)
                tr.dma('sp', rt[:, :, :], WIN[f'l{L}_moe_w_router'].rearrange("(c p) e -> p c e", p=128), writes=[rt])
                tr.op('dve', lambda v, rt=rt, L=L: v.tensor_copy(out=WR[L][:, :, :], in_=rt[:, :, :]), reads=[rt], writes=[WR[L]])
            tr.dma('sp', SINK[:, :], WIN['l3_attn_sink'].partition_broadcast(128), writes=[SINK])
            tr.dma('sp', GROW[:, :], WIN['l1_attn_subln_g'].partition_broadcast(128), writes=[GROW])
            lam_init = 0.8 - 0.6 * math.exp(-0.3 * 1)
            tr.op('dve', lambda v: v.tensor_scalar(out=GROW[:, :], in0=GROW[:, :], scalar1=1.0 - lam_init, scalar2=None,
                                                   op0=ALU.mult), reads=[GROW], writes=[GROW])
            lt = sb(es, "s_lt", [128, 512], F32)
            tr.dma('sp', lt[:, :], WIN['l1_attn_lambda'].rearrange("a d -> (a d)").partition_broadcast(128), writes=[lt])
            pr = sb(es, "s_pr", [128, 256], F32)
            tr.op('dve', lambda v: v.tensor_tensor(out=pr[:, 0:128], in0=lt[:, 0:128], in1=lt[:, 128:256], op=ALU.mult),
                  reads=[lt], writes=[pr])
            tr.op('dve', lambda v: v.tensor_tensor(out=pr[:, 128:256], in0=lt[:, 256:384], in1=lt[:, 384:512], op=ALU.mult),
                  reads=[lt, pr], writes=[pr])
            sm = sb(es, "s_sm", [128, 4], F32)
            tr.op('dve', lambda v: v.tensor_reduce(out=sm[:, 0:2], in_=pr[:, :].rearrange("p (a d) -> p a d", a=2),
                                                   axis=AX.X, op=ALU.add), reads=[pr], writes=[sm])
            tr.op('act', lambda a: a.activation(out=sm[:, 2:4], in_=sm[:, 0:2], func=AF.Exp), reads=[sm], writes=[sm])
            tr.op('dve', lambda v: v.tensor_tensor(out=LAMC[:, :], in0=sm[:, 2:3], in1=sm[:, 3:4], op=ALU.subtract),
                  reads=[sm], writes=[LAMC])
            tr.op('dve', lambda v: v.tensor_scalar(out=LAMC[:, :], in0=LAMC[:, :], scalar1=lam_init, scalar2=None, op0=ALU.add),
                  reads=[LAMC], writes=[LAMC])
            li = vidx['l2_rec_lambda']
            ex = sb(es, "s_ex", [128, 2, 16], F32)
            l1 = sb(es, "s_l1", [128, 2, 16], F32)
            l2 = sb(es, "s_l2", [128, 2, 16], F32)
            mk = sb(es, "s_mk", [128, 2, 16], F32)
            tr.op('act', lambda a: a.activation(out=ex[:, :, :], in_=VEC[:, li:li + 2, :], func=AF.Exp, scale=-1.0),
                  reads=[VEC], writes=[ex])
            tr.op('act', lambda a: a.activation(out=l1[:, :, :], in_=ex[:, :, :], func=AF.Ln, bias=1.0), reads=[ex], writes=[l1])
            tr.op('dve', lambda v: v.tensor_scalar(out=l2[:, :, :], in0=ex[:, :, :], scalar1=-0.2, scalar2=0.25, op0=ALU.mult, op1=ALU.add),
                  reads=[ex], writes=[l2])
            for cst in (1.0 / 3.0, 0.5, 1.0):
                tr.op('dve', lambda v: v.tensor_tensor(out=l2[:, :, :], in0=l2[:, :, :], in1=ex[:, :, :], op=ALU.mult),
                      reads=[l2, ex], writes=[l2])
                tr.op('dve', lambda v, cst=cst: v.tensor_scalar(out=l2[:, :, :], in0=l2[:, :, :], scalar1=-1.0, scalar2=cst,
                                                                 op0=ALU.mult, op1=ALU.add), reads=[l2], writes=[l2])
            tr.op('dve', lambda v: v.tensor_tensor(out=l2[:, :, :], in0=l2[:, :, :], in1=ex[:, :, :], op=ALU.mult),
                  reads=[l2, ex], writes=[l2])
            tr.op('dve', lambda v: v.tensor_single_scalar(out=mk[:, :, :], in_=ex[:, :, :], scalar=0.25, op=ALU.is_gt),
                  reads=[ex], writes=[mk])
            tr.op('dve', lambda v: v.tensor_tensor(out=l1[:, :, :], in0=l1[:, :, :], in1=l2[:, :, :], op=ALU.subtract),
                  reads=[l1, l2], writes=[l1])
            tr.op('dve', lambda v: v.tensor_tensor(out=l1[:, :, :], in0=l1[:, :, :], in1=mk[:, :, :], op=ALU.mult),
                  reads=[l1, mk], writes=[l1])
            tr.op('dve', lambda v: v.tensor_tensor(out=l1[:, :, :], in0=l1[:, :, :], in1=l2[:, :, :], op=ALU.add),
                  reads=[l1, l2], writes=[l1])
            tr.op('dve', lambda v: v.tensor_scalar(out=NSP[:, :, :], in0=l1[:, :, :], scalar1=-8.0, scalar2=None, op0=ALU.mult),
                  reads=[l1], writes=[NSP])
            tr.barrier()

    def convert(names):
        with ExitStack() as es:
            fr = sb(es, "cv_f", [128, 4096], F32, 3)
            br = sb(es, "cv_b", [128, 4096], BF16, 3)
            i = 0
            for n in names:
                R, N, rs, RG, sw = MATS[n]
                src = WIN[n] if rs is None else WIN[n].rearrange(rs)
                dst = WB[n]
                for rb in range(R // 128):
                    g_ = (rb * 128) // RG
                    k_ = ((rb * 128) % RG) // 128
                    for n0 in range(0, N, 4096):
                        nn = min(4096, N - n0)
                        fb = fr.get()
                        bb = br.get()
                        tr.dma('sp', fb[:, 0:nn], src[rb * 128:(rb + 1) * 128, n0:n0 + nn], writes=[fb])
                        if i % 2 == 0:
                            tr.op('dve', lambda v, fb=fb, bb=bb, m=nn: v.tensor_copy(out=bb[:, 0:m], in_=fb[:, 0:m]),
                                  reads=[fb], writes=[bb])
                        else:
                            tr.op('act', lambda a, fb=fb, bb=bb, m=nn: a.copy(out=bb[:, 0:m], in_=fb[:, 0:m]),
                                  reads=[fb], writes=[bb])
                        i += 1
                        dview = dst[g_, n0 // sw:(n0 + nn) // sw, :, k_ * sw:(k_ + 1) * sw].rearrange("s p n -> p s n")
                        tr.dma('pool', dview, bb[:, 0:nn].rearrange("p (s n) -> p s n", n=sw), reads=[bb], writes=[('W', n)])
            tr.barrier()

    def linear(wring, KC, groups, rhs_fn, rhs_reads, epi, order=None, W=TT):
        for gi, grp in enumerate(groups):
            slab = wring.get()
            o = 0
            srcs = []
            for a in grp:
                w = a.shape[1]
                tr.dma('sp', slab[:, o:o + w], a, writes=[slab])
                srcs.append((o, w // KC))
                o += w
            j = 0
            for (o, sw) in srcs:
                for jl in range(sw // 128):
                    ps = PS.get()
                    tr.mm([(ps[:, 0:W], slab[:, o + k * sw + jl * 128:o + k * sw + (jl + 1) * 128], rhs_fn(k), k == 0, k == KC - 1)
                           for k in range(KC)], reads=[slab] + rhs_reads, writes=[ps])
                    epi(gi, j, ps)
                    j += 1

    def layernorm(es_bufs, S, gname, bname, out, silu=False, W=TT, eps=LN_EPS):
        sq, xb, mean_sb, t1, t2 = es_bufs
        mps = PS.get()
        eps_ = PS.get()
        for c in range(C):
            q_ = sq.get()
            x_ = xb.get()
            tr.op('act', lambda a, c=c, q_=q_: a.activation(out=q_[:, 0:W], in_=S[:, c, 0:W], func=AF.Square), reads=[S], writes=[q_])
            tr.op('dve', lambda v, c=c, x_=x_: v.tensor_copy(out=x_[:, 0:W], in_=S[:, c, 0:W]), reads=[S], writes=[x_])
            tr.mm([(mps[:, 0:W], ONESM[:, :], x_[:, 0:W], c == 0, c == C - 1)], reads=[x_, ONESM], writes=[mps])
            tr.mm([(eps_[:, 0:W], ONESM[:, :], q_[:, 0:W], c == 0, c == C - 1)], reads=[q_, ONESM], writes=[eps_])
        tr.op('act', lambda a: a.copy(out=mean_sb[:, 0:W], in_=mps[:, 0:W]), reads=[mps], writes=[mean_sb])
        msq = t1.get()
        tr.op('dve', lambda v: v.tensor_tensor(out=msq[:, 0:W], in0=mean_sb[:, 0:W], in1=mean_sb[:, 0:W], op=ALU.mult),
              reads=[mean_sb], writes=[msq])
        var = t1.get()
        tr.op('dve', lambda v: v.scalar_tensor_tensor(out=var[:, 0:W], in0=eps_[:, 0:W], scalar=eps, in1=msq[:, 0:W],
                                                      op0=ALU.add, op1=ALU.subtract), reads=[eps_, msq], writes=[var])
        rstd = t1.get()
        tr.op('act', lambda a: a.activation(out=rstd[:, 0:W], in_=var[:, 0:W], func=AF.Sqrt), reads=[var], writes=[rstd])
        tr.op('dve', lambda v: v.reciprocal(out=rstd[:, 0:W], in_=rstd[:, 0:W]), reads=[rstd], writes=[rstd])
        gi, bi = vidx[gname], vidx[bname]
        for c in range(C):
            a_ = t2.get()
            tr.op('pool', lambda g, c=c, a_=a_: g.tensor_tensor(out=a_[:, 0:W], in0=S[:, c, 0:W], in1=mean_sb[:, 0:W], op=ALU.subtract),
                  reads=[S, mean_sb], writes=[a_])
            tr.op('dve', lambda v, a_=a_: v.tensor_tensor(out=a_[:, 0:W], in0=a_[:, 0:W], in1=rstd[:, 0:W], op=ALU.mult),
                  reads=[a_, rstd], writes=[a_])
            tr.op('act', lambda a, c=c, a_=a_: a.activation(out=out[:, c, 0:W], in_=a_[:, 0:W], func=(AF.Silu if silu else AF.Identity),
                                                           bias=VEC[:, bi, c:c + 1], scale=VEC[:, gi, c:c + 1]),
                  reads=[a_, VEC], writes=[out])

    def ln_bufs(es, W=TT):
        return (sb(es, "ln_sq", [128, W], BF16, 3), sb(es, "ln_xb", [128, W], BF16, 3), sb(es, "ln_mean", [128, W], F32),
                sb(es, "ln_t1", [128, W], F32, 3), sb(es, "ln_t2", [128, W], F32, 4))

    def fm_view(dram, t0, w, rows0=0, nch=C):
        return dram[rows0:rows0 + nch * 128, t0:t0 + w].rearrange("(c p) t -> p c t", p=128)

    tiles = []
    for si in range(nseq):
        for k in range(seq_lens[si] // TT):
            tiles.append((seq_starts[si] + k * TT, si, seq_starts[si], seq_lens[si]))

    def stage_l0a():
        with ExitStack() as es:
            xin = sb(es, "a_xin", [128, 4, D], F32, 2)
            xb = sb(es, "a_xb", [128, C, TT], BF16, 2)
            gl = sb(es, "a_gl", [128, C, TT], BF16, 2)
            wr = sb(es, "a_w", [128, 8192], BF16, 2)
            sg = sb(es, "a_sg", [128, TT], F32, 4)
            bi = vidx['l0_conv_b_in']
            for (t0, si, s0, sl) in tiles:
                xi = xin.get()
                tr.dma('sp', xi[:, :, :], X[t0:t0 + TT, :].rearrange("(b p) d -> p b d", p=128), writes=[xi])
                xbt = xb.get()
                for c in range(C):
                    ps = PS.get()
                    tr.mm([(ps[:, b * 128:(b + 1) * 128], xi[:, b, c * 128:(c + 1) * 128], IDF[:, :]) for b in range(4)],
                          reads=[xi, IDF], writes=[ps], transpose=True)
                    if c % 2 == 0:
                        tr.op('dve', lambda v, ps=ps, c=c: v.tensor_copy(out=xbt[:, c, :], in_=ps[:, :]), reads=[ps], writes=[xbt])
                    else:
                        tr.op('act', lambda a, ps=ps, c=c: a.copy(out=xbt[:, c, :], in_=ps[:, :]), reads=[ps], writes=[xbt])
                tr.dma('pool', fm_view(XB, t0, TT), xbt[:, :, :], reads=[xbt], writes=[('XB', t0)])
                glt = gl.get()
                wb = WB['l0_conv_w_in']
                groups = [[wb[0, c0 // 2], wb[0, 8 + c0 // 2]] for c0 in range(0, C, 2)]
                hold = {}

                def epi(gi, j, ps):
                    if j < 2:
                        hold[j] = ps
                        return
                    c = 2 * gi + (j - 2)
                    pa = hold.pop(j - 2)
                    s_ = sg.get()
                    tr.op('act', lambda a: a.activation(out=s_[:, :], in_=ps[:, :], func=AF.Sigmoid, bias=VEC[:, bi + 1, c:c + 1]),
                          reads=[ps, VEC], writes=[s_])
                    tr.op('dve', lambda v: v.scalar_tensor_tensor(out=glt[:, c, :], in0=pa[:, :], scalar=VEC[:, bi, c:c + 1], in1=s_[:, :],
                                                                  op0=ALU.add, op1=ALU.mult), reads=[pa, s_, VEC], writes=[glt])
                linear(wr, C, groups, lambda k: xbt[:, k, :], [xbt], epi)
                tr.dma('pool', fm_view(GLU, t0, TT), glt[:, :, :], reads=[glt], writes=[('GLU', t0)])
            tr.barrier()

    def resid_ln(es, name):
        return None

    def stage_l0b():
        HALO = 15
        with ExitStack() as es:
            G = sb(es, "b_g", [128, C, TT + 2 * HALO], BF16, 1)
            S = sb(es, "b_s", [128, C, TT], F32, 2)
            A = sb(es, "b_a", [128, C, TT], BF16, 2)
            wr = sb(es, "b_w", [128, 8192], BF16, 2)
            DGR = sb(es, "b_dg", [128, 31 * 128], BF16, 2)
            tmp = sb(es, "b_tmp", [128, TT], F32, 4)
            lnb = ln_bufs(es)
            wi = vidx['l0_conv_w_dw']
            bdw = vidx['l0_conv_b_dw']
            bo = vidx['l0_conv_b_out']
            for c in range(C):
                dg = DGR.get()
                for k in range(31):
                    if k % 2 == 0:
                        tr.op('dve', lambda v, dg=dg, k=k, c=c: v.tensor_scalar(out=dg[:, k * 128:(k + 1) * 128], in0=IDB[:, :],
                                                                                scalar1=VEC[:, wi + k, c:c + 1], scalar2=None, op0=ALU.mult),
                              reads=[IDB, VEC], writes=[dg])
                    else:
                        tr.op('act', lambda a, dg=dg, k=k, c=c: a.activation(out=dg[:, k * 128:(k + 1) * 128], in_=IDB[:, :], func=AF.Identity,
                                                                             scale=VEC[:, wi + k, c:c + 1]), reads=[IDB, VEC], writes=[dg])
                tr.dma('pool', DIAG[c], dg[:, :], reads=[dg], writes=[('DIAG', c)])
            tr.barrier()
            for (t0, si, s0, sl) in tiles:
                g = G
                lo = max(t0 - HALO, s0)
                hi = min(t0 + TT + HALO, s0 + sl)
                if lo > t0 - HALO:
                    tr.op('pool', lambda p_, g=g: p_.memset(g[:, :, 0:HALO], 0.0), writes=[g])
                if hi < t0 + TT + HALO:
                    tr.op('pool', lambda p_, g=g: p_.memset(g[:, :, TT + HALO:TT + 2 * HALO], 0.0), writes=[g])
                off = lo - (t0 - HALO)
                tr.dma('sp', g[:, :, off:off + (hi - lo)], fm_view(GLU, lo, hi - lo),
                       reads=[('GLU', t) for t in range((lo // TT) * TT, hi, TT)], writes=[g])
                xres = A.get()
                tr.dma('sp', xres[:, :, :], fm_view(XB, t0, TT), reads=[('XB', t0)], writes=[xres])
                s1 = S.get()
                for c in range(C):
                    dg = DGR.get()
                    tr.dma('sp', dg[:, :], DIAG[c], writes=[dg])
                    ps = PS.get()
                    tr.mm([(ps[:, :], dg[:, k * 128:(k + 1) * 128], g[:, c, k:k + TT], k == 0, k == 30) for k in range(31)],
                          reads=[dg, g], writes=[ps])
                    tr.op('act', lambda a, ps=ps, c=c: a.activation(out=s1[:, c, :], in_=ps[:, :], func=AF.Identity, bias=VEC[:, bdw, c:c + 1]),
                          reads=[ps, VEC], writes=[s1])
                hA = A.get()
                layernorm(lnb, s1, 'l0_conv_norm_g', 'l0_conv_norm_b', hA, silu=True)
                s2 = S.get()
                wb = WB['l0_conv_w_out']
                groups = [[wb[0, n0 // 512]] for n0 in range(0, D, 512)]

                def epi(gi, j, ps):
                    c = gi * 4 + j
                    t_ = tmp.get()
                    tr.op('act', lambda a: a.activation(out=t_[:, :], in_=ps[:, :], func=AF.Identity, bias=VEC[:, bo, c:c + 1]),
                          reads=[ps, VEC], writes=[t_])
                    tr.op('dve', lambda v: v.scalar_tensor_tensor(out=s2[:, c, :], in0=xres[:, c, :], scalar=ALPHA, in1=t_[:, :],
                                                                  op0=ALU.mult, op1=ALU.add), reads=[xres, t_], writes=[s2])
                linear(wr, C, groups, lambda k: hA[:, k, :], [hA], epi)
                x1 = A.get()
                layernorm(lnb, s2, 'l0_ln1_g', 'l0_ln1_b', x1)
                tr.dma('pool', fm_view(XB1, t0, TT), x1[:, :, :], reads=[x1], writes=[('XB1', t0)])
            tr.barrier()

    def tail_l1_qkv(es, wr=None):
        oc = sb(es, "t_oc", [128, TT], BF16, 4)
        sqt = sb(es, "t_sq", [128, TT], BF16, 3)
        mx = sb(es, "t_mx", [128, 8], F32, 4)
        vt = sb(es, "t_vt", [128, 512], BF16, 4)
        wb = WB['l1_attn_w_qkv']

        def run(x2, t0, si):
            groups = [[wb[0, n0 // 512]] for n0 in range(0, 4096, 512)]

            def epi(gi, j, ps):
                ch = gi * 4 + j
                o_ = oc.get()
                tr.op('dve', lambda v: v.tensor_copy(out=o_[:, :], in_=ps[:, :]), reads=[ps], writes=[o_])
                tr.dma('pool', QKT[ch * 128:(ch + 1) * 128, t0:t0 + TT], o_[:, :], reads=[o_], writes=[('QKT', t0)])
                if 'n' not in DBG:
                    return
                s_ = sqt.get()
                tr.op('act', lambda a: a.activation(out=s_[:, :], in_=o_[:, :], func=AF.Square), reads=[o_], writes=[s_])
                p2 = PS.get()
                tr.mm([(p2[:, :], ONES1[:, :], s_[:, :], True, True)], reads=[s_, ONES1], writes=[p2])
                m_ = mx.get()
                tr.op('dve', lambda v: v.max(out=m_[:, :], in_=p2[:, :]), reads=[p2], writes=[m_])
                if '2' not in DBG:
                    return
                tr.op('dve', lambda v: v.tensor_tensor(out=NRM[:, si, ch:ch + 1], in0=NRM[:, si, ch:ch + 1], in1=m_[:, 0:1], op=ALU.max),
                      reads=[m_, NRM], writes=[NRM])
            if 'q' in DBG:
                linear(wr, C, groups, lambda k: x2[:, k, :], [x2], epi)
            for n0 in (range(0, 2048, 512) if 'v' in DBG else []):
                slab = wr.get()
                sv = slab[:, 0:C * 512].rearrange("p (k n) -> p k n", k=C)
                tr.dma('sp', slab[:, 0:C * 512], wb[0, 8 + n0 // 512], writes=[slab])
                for b in range(4):
                    ps = PS.get()
                    tr.mm([(ps[:, :], x2[:, k, b * 128:(b + 1) * 128], sv[:, k, :], k == 0, k == C - 1) for k in range(C)],
                          reads=[slab, x2], writes=[ps])
                    v_ = vt.get()
                    tr.op('act', lambda a, ps=ps, v_=v_: a.copy(out=v_[:, :], in_=ps[:, :]), reads=[ps], writes=[v_])
                    tr.dma('pool', VV[t0 + b * 128:t0 + (b + 1) * 128, n0:n0 + 512], v_[:, :], reads=[v_], writes=[('VV', t0)])
        return run

    def tail_l2_win(es, wr=None):
        ob = sb(es, "t_ob", [128, TT], BF16, 4)
        of = sb(es, "t_of", [128, TT], F32, 4)
        wb = WB['l2_rec_w_in']
        bi = vidx['l2_rec_b_in']

        def run(x2, t0, si):
            groups = [[wb[0, n0 // 512]] for n0 in range(0, 4096, 512)]

            def epi(gi, j, ps):
                ch = gi * 4 + j
                if ch < 16:
                    o_ = ob.get()
                    tr.op('act', lambda a: a.activation(out=o_[:, :], in_=ps[:, :], func=AF.Gelu_apprx_tanh, bias=VEC[:, bi, ch:ch + 1]),
                          reads=[ps, VEC], writes=[o_])
                    tr.dma('pool', YB[ch * 128:(ch + 1) * 128, t0:t0 + TT], o_[:, :], reads=[o_], writes=[('YB', t0)])
                else:
                    c = ch - 16
                    o_ = of.get()
                    tr.op('dve', lambda v: v.tensor_scalar(out=o_[:, :], in0=ps[:, :], scalar1=VEC[:, bi + 1, c:c + 1], scalar2=None, op0=ALU.add),
                          reads=[ps, VEC], writes=[o_])
                    tr.dma('pool', RB[c * 128:(c + 1) * 128, t0:t0 + TT], o_[:, :], reads=[o_], writes=[('RB', t0)])
            linear(wr, C, groups, lambda k: x2[:, k, :], [x2], epi)
        return run

    def tail_l3_qkv(es, wr=None):
        oc = sb(es, "t_oc", [128, TT], BF16, 4)
        sqt = sb(es, "t_sq", [128, TT], BF16, 3)
        mx = sb(es, "t_mx", [128, 8], F32, 4)
        vt = sb(es, "t_vt", [128, 512], BF16, 4)
        wb = WB['l3_attn_w_qkv']

        def run(x2, t0, si):
            groups = [[wb[0, n0 // 512]] for n0 in range(0, 2560, 512)]

            def epi(gi, j, ps):
                ch = gi * 4 + j
                o_ = oc.get()
                tr.op('dve', lambda v: v.tensor_copy(out=o_[:, :], in_=ps[:, :]), reads=[ps], writes=[o_])
                if ch < 16:
                    tr.dma('pool', Q3[ch * 128:(ch + 1) * 128, t0:t0 + TT], o_[:, :], reads=[o_], writes=[('Q3', t0)])
                else:
                    tr.dma('pool', K3[(ch - 16) * 128:(ch - 15) * 128, t0:t0 + TT], o_[:, :], reads=[o_], writes=[('K3', t0)])
                s_ = sqt.get()
                tr.op('act', lambda a: a.activation(out=s_[:, :], in_=o_[:, :], func=AF.Square), reads=[o_], writes=[s_])
                p2 = PS.get()
                tr.mm([(p2[:, :], ONES1[:, :], s_[:, :], True, True)], reads=[s_, ONES1], writes=[p2])
                m_ = mx.get()
                tr.op('dve', lambda v: v.max(out=m_[:, :], in_=p2[:, :]), reads=[p2], writes=[m_])
                tr.op('dve', lambda v: v.tensor_tensor(out=NRM3[:, si, ch:ch + 1], in0=NRM3[:, si, ch:ch + 1], in1=m_[:, 0:1], op=ALU.max),
                      reads=[m_, NRM3], writes=[NRM3])
            linear(wr, C, groups, lambda k: x2[:, k, :], [x2], epi)
            slab = wr.get()
            sv = slab[:, 0:C * 512].rearrange("p (k n) -> p k n", k=C)
            tr.dma('sp', slab[:, 0:C * 512], wb[0, 5], writes=[slab])
            for b in range(4):
                ps = PS.get()
                tr.mm([(ps[:, :], x2[:, k, b * 128:(b + 1) * 128], sv[:, k, :], k == 0, k == C - 1) for k in range(C)],
                      reads=[slab, x2], writes=[ps])
                v_ = vt.get()
                tr.op('act', lambda a, ps=ps, v_=v_: a.copy(out=v_[:, :], in_=ps[:, :]), reads=[ps], writes=[v_])
                tr.dma('pool', V3[t0 + b * 128:t0 + (b + 1) * 128, :], v_[:, :], reads=[v_], writes=[('V3', t0)])
        return run

    def tail_store_xb(es, wr=None):
        def run(x2, t0, si):
            pass
        return run

    def tail_final(es, wr=None):
        ot = sb(es, "t_fo", [128, D], F32, 2)

        def run(x2f, t0, si):
            for b in range(4):
                o_ = ot.get()
                for c0 in range(0, C, 4):
                    ps = PS.get()
                    tr.mm([(ps[:, j * 128:(j + 1) * 128], x2f[:, c0 + j, b * 128:(b + 1) * 128], IDF[:, :]) for j in range(4)],
                          reads=[x2f, IDF], writes=[ps], transpose=True)
                    if (c0 // 4) % 2 == 0:
                        tr.op('dve', lambda v, ps=ps, c0=c0, o_=o_: v.tensor_copy(out=o_[:, c0 * 128:(c0 + 4) * 128], in_=ps[:, :]),
                              reads=[ps], writes=[o_])
                    else:
                        tr.op('act', lambda a, ps=ps, c0=c0, o_=o_: a.copy(out=o_[:, c0 * 128:(c0 + 4) * 128], in_=ps[:, :]),
                              reads=[ps], writes=[o_])
                tr.dma('pool', Y[t0 + b * 128:t0 + (b + 1) * 128, :], o_[:, :], reads=[o_], writes=[('Y', t0, b)])
        return run

    def stage_ffn(L, tail_maker, final=False):
        with ExitStack() as es:
            A = sb(es, "f_a", [128, C, TT], BF16, 3)
            H = sb(es, "f_h", [128, 32, TT], BF16)
            S = sb(es, "f_s", [128, C, TT], F32)
            wr = sb(es, "f_w", [128, 8192], BF16, 2)
            tmp = sb(es, "f_tmp", [128, TT], F32, 4)
            lnb = ln_bufs(es)
            tail = tail_maker(es, wr)
            wg, wu, wd = WB[f'l{L}_ffn_w_gate'], WB[f'l{L}_ffn_w_up'], WB[f'l{L}_ffn_w_down']
            for (t0, si, s0, sl) in tiles:
                x1 = A.get()
                tr.dma('sp', x1[:, :, :], fm_view(XB1, t0, TT), reads=[('XB1', t0)], writes=[x1])
                groups = [[wg[0, n0 // 256], wu[0, n0 // 256]] for n0 in range(0, 4096, 256)]
                hold = {}

                def epi(gi, j, ps):
                    if j < 2:
                        hold[j] = ps
                        return
                    hc = 2 * gi + (j - 2)
                    pg = hold.pop(j - 2)
                    s_ = tmp.get()
                    tr.op('act', lambda a: a.activation(out=s_[:, :], in_=pg[:, :], func=AF.Silu), reads=[pg], writes=[s_])
                    tr.op('dve', lambda v: v.tensor_tensor(out=H[:, hc, :], in0=s_[:, :], in1=ps[:, :], op=ALU.mult),
                          reads=[s_, ps], writes=[H])
                linear(wr, C, groups, lambda k: x1[:, k, :], [x1], epi)
                groups = [[wd[0, n0 // 256]] for n0 in range(0, D, 256)]

                def epi2(gi, j, ps):
                    c = gi * 2 + j
                    tr.op('dve', lambda v: v.scalar_tensor_tensor(out=S[:, c, :], in0=x1[:, c, :], scalar=ALPHA, in1=ps[:, :],
                                                                  op0=ALU.mult, op1=ALU.add), reads=[x1, ps], writes=[S])
                linear(wr, 32, groups, lambda k: H[:, k, :], [H], epi2)
                x2 = A.get()
                layernorm(lnb, S, f'l{L}_ln2_g', f'l{L}_ln2_b', x2)
                tr.dma('pool', fm_view(XB, t0, TT), x2[:, :, :], reads=[x2], writes=[('XB', t0)])
                tail(x2, t0, si)
            tr.barrier()

    def stage_moe(L, tail_maker, final=False):
        with ExitStack() as es:
            A = sb(es, "m_a", [128, C, TT], BF16, 1 if final else 2)
            H = sb(es, "m_h", [128, 32, TT], BF16)
            S = sb(es, "m_s", [128, C, TT], F32)
            wr = sb(es, "m_w", [128, 8192], BF16, 2)
            tmp = sb(es, "m_tmp", [128, TT], F32, 4)
            tmp2 = sb(es, "m_tmp2", [128, TT], F32, 4)
            GB = sb(es, "m_gb", [128, 8, TT], BF16)
            LG = sb(es, "m_lg", [128, 4, 8], F32)
            MX = sb(es, "m_mx", [128, 4, 8], F32)
            GT = sb(es, "m_gt", [128, 4, 8], F32)
            E1 = sb(es, "m_e1", [128, 4, 8], F32)
            W2 = sb(es, "m_w2", [128, 4, 2], F32)
            GTS = sb(es, "m_gts", [8, TT], F32)
            lnb = ln_bufs(es)
            tail = tail_maker(es, wr)
            wg, wu, wd = WB[f'l{L}_moe_w_gate'], WB[f'l{L}_moe_w_up'], WB[f'l{L}_moe_w_down']
            wrt = WR[L]
            for (t0, si, s0, sl) in tiles:
                x1 = A.get()
                tr.dma('sp', x1[:, :, :], fm_view(XB1, t0, TT), reads=[('XB1', t0)], writes=[x1])
                ps = PS.get()
                for b in range(4):
                    tr.mm([(ps[:, b * 8:(b + 1) * 8], x1[:, k, b * 128:(b + 1) * 128], wrt[:, k, :], k == 0, k == C - 1) for k in range(C)],
                          reads=[x1, wrt], writes=[ps])
                tr.op('act', lambda a, ps=ps: a.copy(out=LG[:, :, :], in_=ps[:, 0:32].rearrange("p (b e) -> p b e", e=8)),
                      reads=[ps], writes=[LG])
                for b in range(4):
                    tr.op('dve', lambda v, b=b: v.max(out=MX[:, b, :], in_=LG[:, b, :]), reads=[LG], writes=[MX])
                tr.op('dve', lambda v: v.tensor_tensor(out=W2[:, :, 0:1], in0=MX[:, :, 1:2], in1=MX[:, :, 0:1], op=ALU.subtract),
                      reads=[MX], writes=[W2])
                tr.op('act', lambda a: a.activation(out=W2[:, :, 1:2], in_=W2[:, :, 0:1], func=AF.Sigmoid), reads=[W2], writes=[W2])
                tr.op('dve', lambda v: v.tensor_scalar(out=W2[:, :, 0:1], in0=W2[:, :, 1:2], scalar1=-1.0, scalar2=1.0, op0=ALU.mult, op1=ALU.add),
                      reads=[W2], writes=[W2])
                for b in range(4):
                    tr.op('dve', lambda v, b=b: v.tensor_scalar(out=GT[:, b, :], in0=LG[:, b, :], scalar1=MX[:, b, 0:1], scalar2=W2[:, b, 0:1],
                                                                op0=ALU.is_equal, op1=ALU.mult), reads=[LG, MX, W2], writes=[GT])
                    tr.op('dve', lambda v, b=b: v.tensor_scalar(out=E1[:, b, :], in0=LG[:, b, :], scalar1=MX[:, b, 1:2], scalar2=W2[:, b, 1:2],
                                                                op0=ALU.is_equal, op1=ALU.mult), reads=[LG, MX, W2], writes=[E1])
                tr.op('dve', lambda v: v.tensor_tensor(out=GT[:, :, :], in0=GT[:, :, :], in1=E1[:, :, :], op=ALU.add),
                      reads=[GT, E1], writes=[GT])
                ps = PS.get()
                tr.mm([(ps[0:8, b * 128:(b + 1) * 128], GT[:, b, :], IDF[:, :]) for b in range(4)], reads=[GT, IDF], writes=[ps], transpose=True)
                tr.op('act', lambda a, ps=ps: a.copy(out=GTS[:, :], in_=ps[0:8, :]), reads=[ps], writes=[GTS])
                for e in range(8):
                    ps = PS.get()
                    tr.mm([(ps[:, :], SEL[:, e, :], GTS[:, :], True, True)], reads=[SEL, GTS], writes=[ps])
                    tr.op('act', lambda a, ps=ps, e=e: a.copy(out=GB[:, e, :], in_=ps[:, :]), reads=[ps], writes=[GB])
                for eg in range(2):
                    for el in range(4):
                        e = eg * 4 + el
                        groups = [[wg[e, n0 // 256], wu[e, n0 // 256]] for n0 in range(0, 1024, 256)]
                        hold = {}

                        def epi(gi, j, ps, e=e, el=el, hold=hold):
                            if j < 2:
                                hold[j] = ps
                                return
                            hc = el * 8 + 2 * gi + (j - 2)
                            pg = hold.pop(j - 2)
                            s_ = tmp.get()
                            tr.op('act', lambda a: a.activation(out=s_[:, :], in_=pg[:, :], func=AF.Silu), reads=[pg], writes=[s_])
                            u_ = tmp2.get()
                            tr.op('dve', lambda v: v.tensor_tensor(out=u_[:, :], in0=ps[:, :], in1=GB[:, e, :], op=ALU.mult),
                                  reads=[ps, GB], writes=[u_])
                            tr.op('pool', lambda g: g.tensor_tensor(out=H[:, hc, :], in0=s_[:, :], in1=u_[:, :], op=ALU.mult),
                                  reads=[s_, u_], writes=[H])
                        linear(wr, C, groups, lambda k: x1[:, k, :], [x1], epi)
                    groups = [[wd[eg, n0 // 256]] for n0 in range(0, D, 256)]

                    def epi2(gi, j, ps, eg=eg):
                        c = gi * 2 + j
                        if eg == 0:
                            tr.op('dve', lambda v: v.scalar_tensor_tensor(out=S[:, c, :], in0=x1[:, c, :], scalar=ALPHA, in1=ps[:, :],
                                                                          op0=ALU.mult, op1=ALU.add), reads=[x1, ps], writes=[S])
                        else:
                            tr.op('dve', lambda v: v.tensor_tensor(out=S[:, c, :], in0=S[:, c, :], in1=ps[:, :], op=ALU.add),
                                  reads=[S, ps], writes=[S])
                    linear(wr, 32, groups, lambda k: H[:, k, :], [H], epi2)
                if final:
                    layernorm(lnb, S, f'l{L}_ln2_g', f'l{L}_ln2_b', S)
                    tail(S, t0, si)
                else:
                    x2 = A.get()
                    layernorm(lnb, S, f'l{L}_ln2_g', f'l{L}_ln2_b', x2)
                    tr.dma('pool', fm_view(XB, t0, TT), x2[:, :, :], reads=[x2], writes=[('XB', t0)])
                    tail(x2, t0, si)
            tr.barrier()

    def stage_attn_out(L, W=TT):
        with ExitStack() as es:
            A = sb(es, "o_a", [128, C, TT], BF16, 4)
            S = sb(es, "o_s", [128, C, TT], F32, 2)
            wr = sb(es, "o_w", [128, 8192], BF16, 2)
            lnb = ln_bufs(es)
            wb = WB[f'l{L}_attn_w_out']
            for (t0, si, s0, sl) in tiles:
                o = A.get()
                tr.dma('sp', o[:, :, :], fm_view(OT, t0, TT), reads=[('OT', t0)], writes=[o])
                xres = A.get()
                tr.dma('sp', xres[:, :, :], fm_view(XB, t0, TT), reads=[('XB', t0)], writes=[xres])
                s = S.get()
                groups = [[wb[0, n0 // 512]] for n0 in range(0, D, 512)]

                def epi(gi, j, ps):
                    c = gi * 4 + j
                    tr.op('dve', lambda v: v.scalar_tensor_tensor(out=s[:, c, :], in0=xres[:, c, :], scalar=ALPHA, in1=ps[:, :],
                                                                  op0=ALU.mult, op1=ALU.add), reads=[xres, ps], writes=[s])
                linear(wr, C, groups, lambda k: o[:, k, :], [o], epi)
                x1 = A.get()
                layernorm(lnb, s, f'l{L}_ln1_g', f'l{L}_ln1_b', x1)
                tr.dma('pool', fm_view(XB1, t0, TT), x1[:, :, :], reads=[x1], writes=[('XB1', t0)])
            tr.barrier()

    def stage_diff_attn():
        OFF = SMAX - 128
        LS = 2 * SMAX - 128
        scale = 128.0 ** -0.5
        with ExitStack() as es:
            ES = sb(es, "d_es", [128, LS], BF16)
            ti = sb(es, "d_ti", [128, 2048], I32)
            tf = sb(es, "d_tf", [128, 2048], F32)
            KT2 = sb(es, "d_kt", [128, 2, SMAX], BF16)
            QT2 = sb(es, "d_qt", [128, 2, SMAX], BF16)
            VH = sb(es, "d_vh", [128, SMAX // 128, 256], BF16)
            EB = sb(es, "d_e", [128, 2, SMAX], BF16, 2)
            AB = sb(es, "d_a", [128, SMAX], BF16, 2)
            ATB = sb(es, "d_at", [128, SMAX], BF16, 2)
            OTT = sb(es, "d_ot", [128, 2, SMAX], BF16)
            sm = sb(es, "d_sm", [128, 8], F32, 4)
            osb = sb(es, "d_osb", [128, 256], F32, 2)
            junk = sb(es, "d_junk", [128, 256], F32, 2)
            onb = sb(es, "d_onb", [128, 256], BF16, 2)
            negM = sb(es, "d_negm", [128, 2], F32, 2)
            for h in range(8):
                slope = 2.0 ** (-(h + 1))
                for u0 in range(0, LS, 2048):
                    w = min(2048, LS - u0)
                    tr.op('pool', lambda g, u0=u0, w=w: g.iota(ti[:, 0:w], pattern=[[-1, w]], base=OFF - u0, channel_multiplier=1), writes=[ti])
                    tr.op('dve', lambda v, w=w: v.tensor_copy(out=tf[:, 0:w], in_=ti[:, 0:w]), reads=[ti], writes=[tf])
                    tr.op('dve', lambda v, w=w: v.scalar_tensor_tensor(out=tf[:, 0:w], in0=tf[:, 0:w], scalar=-1.0, in1=tf[:, 0:w],
                                                                       op0=ALU.mult, op1=ALU.max), reads=[tf], writes=[tf])
                    tr.op('act', lambda a, u0=u0, w=w, slope=slope: a.activation(out=ES[:, u0:u0 + w], in_=tf[:, 0:w], func=AF.Exp, scale=-slope),
                          reads=[tf], writes=[ES])
                for si in range(nseq):
                    s0, Sl = seq_starts[si], seq_lens[si]
                    nkb = Sl // 128
                    rk = [('QKT', t) for t in range(s0, s0 + Sl, TT)]
                    tr.dma('sp', QT2[:, :, 0:Sl], fm_view(QKT, s0, Sl, rows0=(2 * h) * 128, nch=2), reads=rk, writes=[QT2])
                    tr.dma('sp', KT2[:, :, 0:Sl], fm_view(QKT, s0, Sl, rows0=(16 + 2 * h) * 128, nch=2), reads=rk, writes=[KT2])
                    tr.dma('sp', VH[:, 0:nkb, :], VV[s0:s0 + Sl, h * 256:(h + 1) * 256].rearrange("(kb p) e -> p kb e", p=128),
                           reads=[('VV', t) for t in range(s0, s0 + Sl, TT)], writes=[VH])
                    nm = negM.get()
                    tr.op('dve', lambda v, nm=nm, si=si, h=h: v.tensor_tensor(out=nm[:, :], in0=NRM[:, si, 2 * h:2 * h + 2],
                                                                             in1=NRM[:, si, 16 + 2 * h:16 + 2 * h + 2], op=ALU.mult),
                          reads=[NRM], writes=[nm])
                    tr.op('act', lambda a, nm=nm: a.activation(out=nm[:, :], in_=nm[:, :], func=AF.Sqrt), reads=[nm], writes=[nm])
                    tr.op('dve', lambda v, nm=nm: v.tensor_scalar(out=nm[:, :], in0=nm[:, :], scalar1=-scale, scalar2=None, op0=ALU.mult),
                          reads=[nm], writes=[nm])
                    def phaseA(qb):
                            e_ = EB.get()
                            s_ = sm.get()
                            for c in range(2):
                                for kt in range(Sl // 512):
                                    ps = PS.get()
                                    tr.mm([(ps[:, :], QT2[:, c, qb * 128:(qb + 1) * 128], KT2[:, c, kt * 512:(kt + 1) * 512], True, True)],
                                          reads=[QT2, KT2], writes=[ps])
                                    tr.op('act', lambda a, ps=ps, c=c, kt=kt, e_=e_, nm=nm: a.activation(
                                        out=e_[:, c, kt * 512:(kt + 1) * 512], in_=ps[:, :], func=AF.Exp, bias=nm[:, c:c + 1], scale=scale),
                                        reads=[ps, nm], writes=[(e_, c)])
                                eo = OFF - qb * 128
                                tr.op('dve', lambda v, c=c, e_=e_, s_=s_, eo=eo, Sl=Sl: v.scalar_tensor_tensor(
                                    out=e_[:, c, 0:Sl], in0=e_[:, c, 0:Sl], scalar=1.0, in1=ES[:, eo:eo + Sl], op0=ALU.mult, op1=ALU.mult,
                                    accum_out=s_[:, c:c + 1]), reads=[(e_, c), ES], writes=[(e_, c), (s_, c)])
                            tr.op('dve', lambda v, s_=s_: v.reciprocal(out=s_[:, 2:4], in_=s_[:, 0:2]), reads=[(s_, 0), (s_, 1)], writes=[(s_, 2)])
                            tr.op('dve', lambda v, s_=s_: v.tensor_tensor(out=s_[:, 4:5], in0=s_[:, 0:1], in1=s_[:, 3:4], op=ALU.mult),
                                  reads=[(s_, 2), (s_, 0)], writes=[(s_, 4)])
                            tr.op('dve', lambda v, s_=s_: v.tensor_scalar(out=s_[:, 5:6], in0=s_[:, 4:5], scalar1=LAMC[:, 0:1], scalar2=-1.0,
                                                                          op0=ALU.mult, op1=ALU.mult), reads=[(s_, 4), LAMC], writes=[(s_, 5)])
                            a_ = AB.get()
                            tr.op('dve', lambda v, e_=e_, a_=a_, s_=s_, Sl=Sl: v.scalar_tensor_tensor(
                                out=a_[:, 0:Sl], in0=e_[:, 1, 0:Sl], scalar=s_[:, 5:6], in1=e_[:, 0, 0:Sl], op0=ALU.mult, op1=ALU.add),
                                reads=[(e_, 0), (e_, 1), (s_, 5)], writes=[a_])
                            return (s_, a_)

                    def phaseB(qb, st):
                            s_, a_ = st
                            at = ATB.get()
                            for k0 in range(0, nkb, 8):
                                nk = min(8, nkb - k0)
                                ps = PS.get()
                                psb = ps[:, :].bitcast(BF16)
                                tr.mm([(psb[:, j * 128:(j + 1) * 128], a_[:, (k0 + j) * 128:(k0 + j + 1) * 128], IDB[:, :]) for j in range(nk)],
                                      reads=[a_, IDB], writes=[ps], transpose=True)
                                tr.op('act', lambda a, psb=psb, at=at, k0=k0, nk=nk: a.copy(out=at[:, k0 * 128:(k0 + nk) * 128], in_=psb[:, 0:nk * 128]),
                                      reads=[ps], writes=[(at, k0)])
                            po = PS.get()
                            tr.mm([(po[:, 0:256], at[:, kb * 128:(kb + 1) * 128], VH[:, kb, :], kb == 0, kb == nkb - 1) for kb in range(nkb)],
                                  reads=[(at, k0) for k0 in range(0, nkb, 8)] + [VH], writes=[po])
                            o_ = osb.get()
                            tr.op('act', lambda a, po=po, o_=o_, s_=s_: a.activation(out=o_[:, :], in_=po[:, 0:256], func=AF.Identity, scale=s_[:, 2:3]),
                                  reads=[po, (s_, 2)], writes=[o_])
                            j_ = junk.get()
                            tr.op('act', lambda a, o_=o_, j_=j_, s_=s_: a.activation(out=j_[:, :], in_=o_[:, :], func=AF.Square, accum_out=s_[:, 6:7]),
                                  reads=[o_], writes=[j_, (s_, 6)])
                            tr.op('dve', lambda v, s_=s_: v.tensor_scalar(out=s_[:, 6:7], in0=s_[:, 6:7], scalar1=1.0 / 256.0, scalar2=1e-5,
                                                                          op0=ALU.mult, op1=ALU.add), reads=[(s_, 6)], writes=[(s_, 6)])
                            tr.op('act', lambda a, s_=s_: a.activation(out=s_[:, 7:8], in_=s_[:, 6:7], func=AF.Sqrt), reads=[(s_, 6)], writes=[(s_, 7)])
                            tr.op('dve', lambda v, s_=s_: v.reciprocal(out=s_[:, 7:8], in_=s_[:, 7:8]), reads=[(s_, 7)], writes=[(s_, 7)])
                            on = onb.get()
                            tr.op('dve', lambda v, o_=o_, on=on, s_=s_: v.scalar_tensor_tensor(out=on[:, :], in0=o_[:, :], scalar=s_[:, 7:8], in1=GROW[:, :],
                                                                                             op0=ALU.mult, op1=ALU.mult),
                                  reads=[o_, (s_, 7), GROW], writes=[on])
                            ps = PS.get()
                            psb = ps[:, :].bitcast(BF16)
                            tr.mm([(psb[:, j * 128:(j + 1) * 128], on[:, j * 128:(j + 1) * 128], IDB[:, :]) for j in range(2)],
                                  reads=[on, IDB], writes=[ps], transpose=True)
                            tr.op('act', lambda a, psb=psb, qb=qb: a.copy(out=OTT[:, :, qb * 128:(qb + 1) * 128],
                                                                           in_=psb[:, 0:256].rearrange("p (j q) -> p j q", j=2)),
                                  reads=[ps], writes=[OTT])

                    st_ = phaseA(0)
                    for qb in range(nkb):
                        nx_ = phaseA(qb + 1) if qb + 1 < nkb else None
                        phaseB(qb, st_)
                        st_ = nx_
                    tr.dma('pool', fm_view(OT, s0, Sl, rows0=2 * h * 128, nch=2), OTT[:, :, 0:Sl], reads=[OTT],
                           writes=[('OT', t) for t in range(s0, s0 + Sl, TT)])
            tr.barrier()

    def stage_win_attn():
        scale = 128.0 ** -0.5
        with ExitStack() as es:
            EW = sb(es, "w_ew", [128, 16, 384], BF16)
            ti = sb(es, "w_ti", [128, 384], I32)
            tf = sb(es, "w_tf", [128, 384], F32)
            tm = sb(es, "w_tm", [128, 384], F32)
            te = sb(es, "w_te", [128, 384], F32)
            QT = sb(es, "w_q", [128, 16, TT], BF16, 2)
            KW = sb(es, "w_k", [128, 4, TT + 256], BF16, 2)
            VW = sb(es, "w_v", [128, 6, 512], BF16, 2)
            EBf = sb(es, "w_e", [128, 384], BF16, 3)
            PT = sb(es, "w_pt", [128, 384], BF16, 3)
            OB = sb(es, "w_ob", [128, D], BF16, 2)
            OTt = sb(es, "w_ot", [128, C, TT], BF16, 2)
            sm = sb(es, "w_sm", [128, 4], F32, 6)
            NM = sb(es, "w_nm", [128, nseq, 16], F32)
            SK = sb(es, "w_sk", [128, nseq, 16], F32)
            tr.op('pool', lambda g: g.iota(ti[:, :], pattern=[[-1, 384]], base=128, channel_multiplier=1), writes=[ti])
            tr.op('dve', lambda v: v.tensor_copy(out=tf[:, :], in_=ti[:, :]), reads=[ti], writes=[tf])
            tr.op('dve', lambda v: v.scalar_tensor_tensor(out=tf[:, :], in0=tf[:, :], scalar=-1.0, in1=tf[:, :], op0=ALU.mult, op1=ALU.max),
                  reads=[tf], writes=[tf])
            tr.op('dve', lambda v: v.tensor_single_scalar(out=tm[:, :], in_=tf[:, :], scalar=128.0, op=ALU.is_le), reads=[tf], writes=[tm])
            for h in range(16):
                slope = 2.0 ** (-8.0 * (h + 1) / 16.0)
                tr.op('act', lambda a, slope=slope: a.activation(out=te[:, :], in_=tf[:, :], func=AF.Exp, scale=-slope), reads=[tf], writes=[te])
                tr.op('dve', lambda v, h=h: v.tensor_tensor(out=EW[:, h, :], in0=te[:, :], in1=tm[:, :], op=ALU.mult), reads=[te, tm], writes=[EW])
            for si in range(nseq):
                for kv in range(4):
                    tr.op('dve', lambda v, si=si, kv=kv: v.tensor_scalar(out=NM[:, si, kv * 4:(kv + 1) * 4], in0=NRM3[:, si, kv * 4:(kv + 1) * 4],
                                                                          scalar1=NRM3[:, si, 16 + kv:17 + kv], scalar2=None, op0=ALU.mult),
                          reads=[NRM3], writes=[NM])
            tr.op('act', lambda a: a.activation(out=NM[:, :, :], in_=NM[:, :, :], func=AF.Sqrt), reads=[NM], writes=[NM])
            tr.op('dve', lambda v: v.tensor_scalar(out=NM[:, :, :], in0=NM[:, :, :], scalar1=-scale, scalar2=None, op0=ALU.mult),
                  reads=[NM], writes=[NM])
            for si in range(nseq):
                tr.op('dve', lambda v, si=si: v.tensor_tensor(out=SK[:, si, :], in0=NM[:, si, :], in1=SINK[:, :], op=ALU.add),
                      reads=[NM, SINK], writes=[SK])
            tr.op('act', lambda a: a.activation(out=SK[:, :, :], in_=SK[:, :, :], func=AF.Exp), reads=[SK], writes=[SK])
            for (t0, si, s0, sl) in tiles:
                q = QT.get()
                tr.dma('sp', q[:, :, :], fm_view(Q3, t0, TT), reads=[('Q3', t0)], writes=[q])
                lo = max(t0 - 128, s0)
                hi = min(t0 + TT + 128, s0 + sl)
                off = lo - (t0 - 128)
                kw = KW.get()
                rk = [t for t in range((lo // TT) * TT, hi, TT)]
                tr.dma('sp', kw[:, :, off:off + hi - lo], fm_view(K3, lo, hi - lo, nch=4), reads=[('K3', t) for t in rk], writes=[kw])
                vw = VW.get()
                tr.dma('sp', vw[:, off // 128:(off + hi - lo) // 128, :], V3[lo:hi, :].rearrange("(b p) e -> p b e", p=128),
                       reads=[('V3', t) for t in rk], writes=[vw])
                ott = OTt.get()
                for b in range(4):
                    g0 = t0 + b * 128
                    c_lo = b * 128 + (128 if g0 - 128 < s0 else 0)
                    c_hi = b * 128 + 384 - (128 if g0 + 256 > s0 + sl else 0)
                    Wd = c_hi - c_lo
                    eo = c_lo - b * 128
                    ob = OB.get()
                    for h in range(16):
                        kv = h // 4
                        ps = PS.get()
                        tr.mm([(ps[:, 0:Wd], q[:, h, b * 128:(b + 1) * 128], kw[:, kv, c_lo:c_hi], True, True)], reads=[q, kw], writes=[ps])
                        e_ = EBf.get()
                        s_ = sm.get()
                        tr.op('act', lambda a, ps=ps, e_=e_, Wd=Wd, si=si, h=h: a.activation(out=e_[:, 0:Wd], in_=ps[:, 0:Wd], func=AF.Exp,
                                                                                           bias=NM[:, si, h:h + 1], scale=scale),
                              reads=[ps, NM], writes=[e_])
                        tr.op('dve', lambda v, e_=e_, s_=s_, Wd=Wd, eo=eo, h=h: v.scalar_tensor_tensor(
                            out=e_[:, 0:Wd], in0=e_[:, 0:Wd], scalar=1.0, in1=EW[:, h, eo:eo + Wd], op0=ALU.mult, op1=ALU.mult,
                            accum_out=s_[:, 0:1]), reads=[e_, EW], writes=[e_, s_])
                        tr.op('dve', lambda v, s_=s_, si=si, h=h: v.tensor_tensor(out=s_[:, 1:2], in0=s_[:, 0:1], in1=SK[:, si, h:h + 1], op=ALU.add),
                              reads=[s_, SK], writes=[s_])
                        tr.op('dve', lambda v, s_=s_: v.reciprocal(out=s_[:, 2:3], in_=s_[:, 1:2]), reads=[s_], writes=[s_])
                        nb_ = Wd // 128
                        pt = PT.get()
                        ps2 = PS.get()
                        psb = ps2[:, :].bitcast(BF16)
                        tr.mm([(psb[:, j * 128:(j + 1) * 128], e_[:, j * 128:(j + 1) * 128], IDB[:, :]) for j in range(nb_)],
                              reads=[e_, IDB], writes=[ps2], transpose=True)
                        tr.op('act', lambda a, psb=psb, pt=pt, Wd=Wd: a.copy(out=pt[:, 0:Wd], in_=psb[:, 0:Wd]), reads=[ps2], writes=[pt])
                        po = PS.get()
                        tr.mm([(po[:, 0:128], pt[:, j * 128:(j + 1) * 128], vw[:, c_lo // 128 + j, kv * 128:(kv + 1) * 128], j == 0, j == nb_ - 1)
                               for j in range(nb_)], reads=[pt, vw], writes=[po])
                        tr.op('act', lambda a, po=po, ob=ob, s_=s_, h=h: a.activation(out=ob[:, h * 128:(h + 1) * 128], in_=po[:, 0:128],
                                                                                      func=AF.Identity, scale=s_[:, 2:3]),
                              reads=[po, s_], writes=[ob])
                    for c0 in range(0, C, 8):
                        ps = PS.get()
                        psb = ps[:, :].bitcast(BF16)
                        tr.mm([(psb[:, j * 128:(j + 1) * 128], ob[:, (c0 + j) * 128:(c0 + j + 1) * 128], IDB[:, :]) for j in range(8)],
                              reads=[ob, IDB], writes=[ps], transpose=True)
                        tr.op('dve', lambda v, psb=psb, c0=c0, b=b: v.tensor_copy(out=ott[:, c0:c0 + 8, b * 128:(b + 1) * 128],
                                                                                  in_=psb[:, :].rearrange("p (j q) -> p j q", j=8)),
                              reads=[ps], writes=[ott])
                tr.dma('pool', fm_view(OT, t0, TT), ott[:, :, :], reads=[ott], writes=[('OT', t0)])
            tr.barrier()

    def stage_lru(dir_):
        TR = 256
        with ExitStack() as es:
            RT = sb(es, "r_rt", [128, C, TR + 3], F32, 1)
            RC = sb(es, "r_rc", [128, C, TR], F32, 1)
            RCB = sb(es, "r_rcb", [128, C, TR], BF16, 1)
            HS = sb(es, "r_hs", [128, C, TR], F32, 2)
            GS = sb(es, "r_gs", [128, 16, 256], BF16, 2)
            tg = sb(es, "r_tg", [128, TR], F32, 3)
            ta = sb(es, "r_ta", [128, TR], F32, 3)
            tb = sb(es, "r_tb", [128, TR], F32, 3)
            if dir_ == 1:
                wr = sb(es, "r_w", [128, 4096], BF16, 2)
                HFT = sb(es, "r_hf", [128, C, TR], F32, 1)
                YBT = sb(es, "r_yb", [128, C, TR], BF16, 1)
                MT = sb(es, "r_mt", [128, C, TR], BF16, 1)
                XR = sb(es, "r_xr", [128, C, TR], BF16, 1)
                S = sb(es, "r_s", [128, C, TR], F32, 1)
                X1 = sb(es, "r_x1", [128, C, TR], BF16, 1)
                tmp = sb(es, "r_tmp", [128, TR], F32, 2)
                lnb = ln_bufs(es, TR)
            cw = vidx['l2_rec_conv_w']
            cb = vidx['l2_rec_conv_b']
            gb = vidx['l2_rec_gate_b']
            bo = vidx['l2_rec_b_out']
            gw = WB['l2_rec_gate_w']
            for si in range(nseq):
                s0, Sl = seq_starts[si], seq_lens[si]
                nt = Sl // TR
                order = range(nt) if dir_ == 0 else range(nt - 1, -1, -1)
                prev = None
                for ti_ in order:
                    t0 = s0 + ti_ * TR
                    rt = RT.get()
                    lo = max(t0 - 2, s0)
                    hi = min(t0 + TR + 1, s0 + Sl)
                    if lo > t0 - 2:
                        tr.op('pool', lambda g, rt=rt: g.memset(rt[:, :, 0:2], 0.0), writes=[rt])
                    if hi < t0 + TR + 1:
                        tr.op('pool', lambda g, rt=rt: g.memset(rt[:, :, TR + 2:TR + 3], 0.0), writes=[rt])
                    off = lo - (t0 - 2)
                    tr.dma('sp', rt[:, :, off:off + hi - lo], fm_view(RB, lo, hi - lo),
                           reads=[('RB', t) for t in range((lo // TT) * TT, hi, TT)], writes=[rt])
                    rc = RC.get()
                    rcb = RCB.get()
                    for c in range(C):
                        tr.op('dve', lambda v, c=c: v.tensor_scalar(out=rc[:, c, :], in0=rt[:, c, 0:TR], scalar1=VEC[:, cw, c:c + 1],
                                                                    scalar2=VEC[:, cb, c:c + 1], op0=ALU.mult, op1=ALU.add),
                              reads=[rt, VEC], writes=[(rc, c)])
                    for k in range(1, 4):
                        for c in range(C):
                            tr.op('dve', lambda v, c=c, k=k: v.scalar_tensor_tensor(out=rc[:, c, :], in0=rt[:, c, k:k + TR], scalar=VEC[:, cw + k, c:c + 1],
                                                                                   in1=rc[:, c, :], op0=ALU.mult, op1=ALU.add),
                                  reads=[rt, VEC, (rc, c)], writes=[(rc, c)])
                    for c in range(C):
                        tr.op('pool', lambda g, c=c: g.tensor_copy(out=rcb[:, c, :], in_=rc[:, c, :]), reads=[(rc, c)], writes=[(rcb, c)])
                    hs = HS.get()
                    slabs = []
                    for g_ in range(2):
                        sl_ = GS.get()
                        base = (dir_ * 2 + g_) * 2048
                        tr.dma('sp', sl_[:, :, :], gw[dir_ * 2 + g_, 0].rearrange("p (j o) -> p j o", o=256), writes=[sl_])
                        slabs.append(sl_)
                    for oc in range(C):
                        n_, ol = oc // 2, oc % 2
                        pss = []
                        for g_ in range(2):
                            ps = PS.get()
                            tr.mm([(ps[:, 0:TR], slabs[g_][:, n_ * 2 + kc, ol * 128:(ol + 1) * 128], rcb[:, 2 * n_ + kc, :], kc == 0, kc == 1) for kc in range(2)],
                                  reads=[slabs[g_], (rcb, 2 * n_), (rcb, 2 * n_ + 1)], writes=[ps])
                            pss.append(ps)
                        r_ = tg.get()
                        tr.op('act', lambda a, r_=r_, ps=pss[0], oc=oc: a.activation(out=r_[:, :], in_=ps[:, 0:TR], func=AF.Sigmoid,
                                                                                   bias=VEC[:, gb + dir_ * 2, oc:oc + 1]), reads=[pss[0], VEC], writes=[r_])
                        a_ = ta.get()
                        tr.op('act', lambda a, r_=r_, a_=a_, oc=oc: a.activation(out=a_[:, :], in_=r_[:, :], func=AF.Exp, scale=NSP[:, dir_, oc:oc + 1]),
                              reads=[r_, NSP], writes=[a_])
                        i_ = tg.get()
                        tr.op('act', lambda a, i_=i_, ps=pss[1], oc=oc: a.activation(out=i_[:, :], in_=ps[:, 0:TR], func=AF.Sigmoid,
                                                                                   bias=VEC[:, gb + dir_ * 2 + 1, oc:oc + 1]), reads=[pss[1], VEC], writes=[i_])
                        q_ = tb.get()
                        tr.op('dve', lambda v, q_=q_, a_=a_: v.scalar_tensor_tensor(out=q_[:, :], in0=a_[:, :], scalar=-1.0, in1=a_[:, :], op0=ALU.mult, op1=ALU.mult),
                              reads=[a_], writes=[q_])
                        tr.op('dve', lambda v, q_=q_: v.tensor_scalar(out=q_[:, :], in0=q_[:, :], scalar1=1.0, scalar2=1e-30, op0=ALU.add, op1=ALU.max),
                              reads=[q_], writes=[q_])
                        tr.op('act', lambda a, q_=q_: a.activation(out=q_[:, :], in_=q_[:, :], func=AF.Sqrt), reads=[q_], writes=[q_])
                        tr.op('dve', lambda v, i_=i_, oc=oc: v.tensor_tensor(out=i_[:, :], in0=i_[:, :], in1=rc[:, oc, :], op=ALU.mult),
                              reads=[i_, (rc, oc)], writes=[i_])
                        tr.op('dve', lambda v, i_=i_, q_=q_: v.tensor_tensor(out=i_[:, :], in0=i_[:, :], in1=q_[:, :], op=ALU.mult),
                              reads=[i_, q_], writes=[i_])
                        if dir_ == 0:
                            init = 0.0 if prev is None else prev[:, oc, TR - 1:TR]
                            tr.op('dve', lambda v, a_=a_, i_=i_, oc=oc, init=init: v.tensor_tensor_scan(out=hs[:, oc, :], data0=a_[:, :], data1=i_[:, :],
                                                                                                   initial=init, op0=ALU.mult, op1=ALU.add),
                                  reads=[a_, i_] + ([] if prev is None else [(prev, oc)]), writes=[(hs, oc)])
                        else:
                            init = 0.0 if prev is None else prev[:, oc, 0:1]
                            tr.op('dve', lambda v, a_=a_, i_=i_, oc=oc, init=init: v.tensor_tensor_scan(out=hs[:, oc, ::-1], data0=a_[:, ::-1], data1=i_[:, ::-1],
                                                                                                   initial=init, op0=ALU.mult, op1=ALU.add),
                                  reads=[a_, i_] + ([] if prev is None else [(prev, oc)]), writes=[(hs, oc)])
                    hkeys = [(hs, c) for c in range(C)]
                    if dir_ == 0:
                        tr.dma('pool', fm_view(HF, t0, TR), hs[:, :, :], reads=hkeys, writes=[('HF', t0)])
                    else:
                        hf = HFT.get()
                        tr.dma('sp', hf[:, :, :], fm_view(HF, t0, TR), reads=[('HF', t0)], writes=[hf])
                        yb = YBT.get()
                        tr.dma('sp', yb[:, :, :], fm_view(YB, t0, TR), reads=[('YB', (t0 // TT) * TT)], writes=[yb])
                        xr = XR.get()
                        tr.dma('sp', xr[:, :, :], fm_view(XB, t0, TR), reads=[('XB', (t0 // TT) * TT)], writes=[xr])
                        mt = MT.get()
                        for c in range(C):
                            tr.op('pool', lambda g, c=c, hf=hf: g.tensor_tensor(out=hf[:, c, :], in0=hf[:, c, :], in1=hs[:, c, :], op=ALU.add),
                                  reads=[hf, (hs, c)], writes=[hf])
                            tr.op('dve', lambda v, c=c, hf=hf, yb=yb, mt=mt: v.tensor_tensor(out=mt[:, c, :], in0=hf[:, c, :], in1=yb[:, c, :], op=ALU.mult),
                                  reads=[hf, yb], writes=[mt])
                        s = S.get()
                        wb = WB['l2_rec_w_out']
                        groups = [[wb[0, n0 // 256]] for n0 in range(0, D, 256)]

                        def epi(gi, j, ps, xr=xr, s=s):
                            c = gi * 2 + j
                            t_ = tmp.get()
                            tr.op('act', lambda a: a.activation(out=t_[:, :], in_=ps[:, 0:TR], func=AF.Identity, bias=VEC[:, bo, c:c + 1]),
                                  reads=[ps, VEC], writes=[t_])
                            tr.op('dve', lambda v: v.scalar_tensor_tensor(out=s[:, c, :], in0=xr[:, c, :], scalar=ALPHA, in1=t_[:, :],
                                                                          op0=ALU.mult, op1=ALU.add), reads=[xr, t_], writes=[s])
                        linear(wr, C, groups, lambda k, mt=mt: mt[:, k, :], [mt], epi, W=TR)
                        x1 = X1.get()
                        layernorm(lnb, s, 'l2_ln1_g', 'l2_ln1_b', x1, W=TR)
                        tr.dma('pool', fm_view(XB1, t0, TR), x1[:, :, :], reads=[x1], writes=[('XB1', (t0 // TT) * TT, t0)])
                    prev = hs
            tr.barrier()

    def stage_dump(src):
        with ExitStack() as es:
            A = sb(es, "z_a", [128, C, TT], BF16, 2)
            AF_ = sb(es, "z_f", [128, C, TT], F32, 2)
            tail = tail_final(es)
            for (t0, si, s0, sl) in tiles:
                a = A.get()
                tr.dma('sp', a[:, :, :], fm_view(src, t0, TT), writes=[a])
                f = AF_.get()
                tr.op('dve', lambda v, a=a, f=f: v.tensor_copy(out=f[:, :, :], in_=a[:, :, :]), reads=[a], writes=[f])
                tail(f, t0, si)
            tr.barrier()

    setup()
    L0 = ['l0_conv_w_in', 'l0_conv_w_out', 'l0_ffn_w_gate', 'l0_ffn_w_up', 'l0_ffn_w_down', 'l1_attn_w_qkv']
    L1 = ['l1_attn_w_out', 'l1_moe_w_gate', 'l1_moe_w_up', 'l1_moe_w_down', 'l2_rec_w_in']
    L2 = ['l2_rec_gate_w', 'l2_rec_w_out', 'l2_ffn_w_gate', 'l2_ffn_w_up', 'l2_ffn_w_down', 'l3_attn_w_qkv']
    L3 = ['l3_attn_w_out', 'l3_moe_w_gate', 'l3_moe_w_up', 'l3_moe_w_down']
    convert(L0)
    stage_l0a()
    stage_l0b()
    def prog():
        if n_layers == 1:
            stage_ffn(0, tail_store_xb)
            return stage_dump(XB)
        stage_ffn(0, tail_l1_qkv)
        if n_layers == 1.25:
            return stage_dump(XB)
        convert(L1)
        stage_diff_attn()
        if n_layers == 1.5:
            return stage_dump(OT)
        stage_attn_out(1)
        if n_layers == 1.75:
            return stage_dump(XB1)
        if n_layers == 2:
            stage_moe(1, tail_store_xb)
            return stage_dump(XB)
        stage_moe(1, tail_l2_win)
        convert(L2)
        stage_lru(0)
        if n_layers == 2.5:
            return stage_dump(XB)
        stage_lru(1)
        if n_layers == 2.75:
            return stage_dump(XB1)
        if n_layers == 3:
            stage_ffn(2, tail_store_xb)
            return stage_dump(XB)
        stage_ffn(2, tail_l3_qkv)
        convert(L3)
        stage_win_attn()
        if n_layers == 3.5:
            return stage_dump(OT)
        stage_attn_out(3)
        if n_layers == 3.75:
            return stage_dump(XB1)
        stage_moe(3, tail_final, final=True)
    prog()
    tr.barrier(engines=['sp'])
    return nc, tr


_CACHE = {}


def run_cores(x_cores, weights, seq_lens, n_layers=4, n_cores=N_CORES, trace=False):
    key = (tuple(seq_lens), n_layers)
    nc, tr = build(list(seq_lens), n_layers)
    in_maps = []
    for ci in range(n_cores):
        m = {"x": np.ascontiguousarray(x_cores[ci])}
        for n in WSHAPES:
            m[n] = weights[n]
        in_maps.append(m)
    res = run_bass_kernel_spmd(nc, in_maps, core_ids=list(range(n_cores)), trace=trace)
    return [r["y"] for r in res.results], res


def kernel(**inputs):
    xp = np.asarray(inputs['x_prompt'], dtype=np.float32)
    xs = np.asarray(inputs['x_sample'], dtype=np.float32)
    weights = {n: np.ascontiguousarray(np.asarray(inputs[n], dtype=np.float32)) for n in WSHAPES}
    x_cores = []
    for ci in range(N_CORES):
        x_cores.append(np.concatenate([xp[2 * ci].reshape(-1, D), xp[2 * ci + 1].reshape(-1, D), xs[ci].reshape(-1, D)], axis=0))
    ys, _ = run_cores(x_cores, weights, [2048, 2048, 4096])
    yp = np.empty_like(xp)
    ysamp = np.empty_like(xs)
    for ci in range(N_CORES):
        y = ys[ci]
        yp[2 * ci] = y[0:2048]
        yp[2 * ci + 1] = y[2048:4096]
        ysamp[ci] = y[4096:8192]
    return (yp, ysamp)
```
